# Optimizing a Trainium2 kernel written in Bass

```python
import math
import jax, jax.numpy as jnp
from jax import lax
import numpy as np

D_MODEL = 4096
BATCH = 4
SEQ = 2048
DEPTH = 1

HEAD_DIM = 128
N_HEADS_A = 16
N_HEADS_B = 16
WA = N_HEADS_A * HEAD_DIM
WB = N_HEADS_B * HEAD_DIM
IDX_HEADS = 16
IDX_DIM = 64
TOPK_MAX = 256
Q_BLOCK = 128
N_BUCKETS = 32
MAX_DISTANCE = 128
D_FF = 11008
N_MOD = 9
RMS_EPS = 1e-6
FORGET_BIAS_MEAN = 2.0
D_IN = WA + 2 * HEAD_DIM + IDX_HEADS * IDX_DIM + IDX_DIM + IDX_HEADS + 3 * WB + N_HEADS_B + 2 * D_MODEL

kernel_name = 'hybrid_dsa_fox_macaron_adaln_block'


def _split_points():
    sizes = (WA, HEAD_DIM, HEAD_DIM, IDX_HEADS * IDX_DIM, IDX_DIM, IDX_HEADS,
             WB, WB, WB, N_HEADS_B, D_MODEL, D_MODEL)
    return tuple(int(v) for v in np.cumsum(sizes)[:-1])


def rms_norm(x, g):
    xf = x.astype(jnp.float32)
    y = xf * lax.rsqrt(jnp.mean(xf * xf, axis=-1, keepdims=True) + RMS_EPS)
    return (y * g.astype(jnp.float32)).astype(x.dtype)


def modulate(h, shift, scale):
    return h * (1.0 + scale[:, None, :]) + shift[:, None, :]


def swiglu(h, w_in, w_out):
    a, b = jnp.split(h @ w_in, 2, axis=-1)
    return (jax.nn.silu(a) * b) @ w_out


def t5_bucket(dist):
    n = jnp.maximum(dist, 0)
    max_exact = N_BUCKETS // 2
    nf = jnp.maximum(n, max_exact).astype(jnp.float32)
    large = max_exact + (jnp.log(nf / max_exact) / math.log(MAX_DISTANCE / max_exact)
                         * (N_BUCKETS - max_exact)).astype(jnp.int32)
    large = jnp.minimum(large, N_BUCKETS - 1)
    return jnp.where(n < max_exact, n, large)


def _to_blocks(a):
    B, L = a.shape[:2]
    return jnp.moveaxis(a.reshape(B, L // Q_BLOCK, Q_BLOCK, *a.shape[2:]), 1, 0)


def _from_blocks(a):
    nb, B, q = a.shape[:3]
    return jnp.moveaxis(a, 0, 1).reshape(B, nb * q, *a.shape[3:])


_gather_rows = jax.vmap(lambda table, idx: table[idx])


def dsa_attention(q, k, v, q_idx, k_idx, w_idx, rel_bias):
    B, L = q.shape[:2]
    topk = min(TOPK_MAX, L // 4)
    pos = jnp.arange(L, dtype=jnp.int32)
    scale = HEAD_DIM ** -0.5
    w_idx = w_idx * (IDX_HEADS ** -0.5 * IDX_DIM ** -0.5)

    def block(args):
        qb, qib, wb, tb = args
        s = jax.nn.relu(jnp.einsum('bqhd,bsd->bqhs', qib, k_idx).astype(jnp.float32))
        score = jnp.einsum('bqhs,bqh->bqs', s, wb.astype(jnp.float32))
        causal = pos[None, :] <= tb[:, None]
        score = jnp.where(causal[None], score, -jnp.inf)
        _, idx = lax.top_k(score, topk)
        k_sel = _gather_rows(k, idx)
        v_sel = _gather_rows(v, idx)
        dist = tb[None, :, None] - idx
        bias = jnp.moveaxis(rel_bias[t5_bucket(dist)], -1, 1)
        logits = (jnp.einsum('bqhd,bqkd->bhqk', qb, k_sel).astype(jnp.float32) * scale
                  + bias.astype(jnp.float32))
        logits = jnp.where((dist >= 0)[:, None], logits, -jnp.inf)
        p = jax.nn.softmax(logits, axis=-1).astype(v.dtype)
        return jnp.einsum('bhqk,bqkd->bqhd', p, v_sel)

    out = lax.map(block, (_to_blocks(q), _to_blocks(q_idx), _to_blocks(w_idx),
                          pos.reshape(-1, Q_BLOCK)))
    return _from_blocks(out).reshape(B, L, WA)


def forgetting_attention(q, k, v, f_logit):
    B, L = q.shape[:2]
    pos = jnp.arange(L, dtype=jnp.int32)
    scale = HEAD_DIM ** -0.5
    F = lax.cumsum(jax.nn.log_sigmoid(f_logit.astype(jnp.float32)), axis=1)
    F_key = jnp.moveaxis(F, -1, 1)[:, :, None, :]

    def block(args):
        qb, Fq, tb = args
        logits = jnp.einsum('bqhd,bshd->bhqs', qb, k).astype(jnp.float32) * scale
        logits = logits + jnp.moveaxis(Fq, -1, 1)[..., None] - F_key
        causal = pos[None, :] <= tb[:, None]
        logits = jnp.where(causal[None, None], logits, -jnp.inf)
        p = jax.nn.softmax(logits, axis=-1).astype(v.dtype)
        return jnp.einsum('bhqs,bshd->bqhd', p, v)

    out = lax.map(block, (_to_blocks(q), _to_blocks(F), pos.reshape(-1, Q_BLOCK)))
    return _from_blocks(out).reshape(B, L, WB)


def hybrid_mixer(h, w_in, b_forget, rel_bias, w_up_a, w_up_b, w_o):
    B, L, _ = h.shape
    proj = h @ w_in
    (q_a, k_a, v_a, q_i, k_i, w_i, q_b, k_b, v_b, f_b, gate_a, gate_b) = jnp.split(
        proj, _split_points(), axis=-1)
    o_a = dsa_attention(q_a.reshape(B, L, N_HEADS_A, HEAD_DIM), k_a, v_a,
                        q_i.reshape(B, L, IDX_HEADS, IDX_DIM), k_i, w_i, rel_bias)
    o_b = forgetting_attention(q_b.reshape(B, L, N_HEADS_B, HEAD_DIM),
                               k_b.reshape(B, L, N_HEADS_B, HEAD_DIM),
                               v_b.reshape(B, L, N_HEADS_B, HEAD_DIM),
                               f_b + b_forget)
    merged = jax.nn.sigmoid(gate_a) * (o_a @ w_up_a) + jax.nn.sigmoid(gate_b) * (o_b @ w_up_b)
    return merged @ w_o


def setup_inputs(seed: int = 0) -> dict:
    key = jax.random.key(seed)
    ks = jax.random.split(key, 18)
    f32 = jnp.float32

    def dense(k, shape, fan_in):
        return jax.random.normal(k, shape, f32) * fan_in ** -0.5

    def gain(k, shape):
        return 1.0 + 0.02 * jax.random.normal(k, shape, f32)

    return {
        'x': jax.random.normal(ks[0], (BATCH, SEQ, D_MODEL), f32),
        'c': jax.random.normal(ks[1], (BATCH, D_MODEL), f32),
        'w_ada': dense(ks[2], (DEPTH, D_MODEL, N_MOD * D_MODEL), D_MODEL),
        'b_ada': 0.02 * jax.random.normal(ks[3], (DEPTH, N_MOD * D_MODEL), f32),
        'g_ffn1': gain(ks[4], (DEPTH, D_MODEL)),
        'ffn1_w_in': dense(ks[5], (DEPTH, D_MODEL, 2 * D_FF), D_MODEL),
        'ffn1_w_out': dense(ks[6], (DEPTH, D_FF, D_MODEL), D_FF),
        'g_mix': gain(ks[7], (DEPTH, D_MODEL)),
        'w_in': dense(ks[8], (DEPTH, D_MODEL, D_IN), D_MODEL),
        'b_forget': FORGET_BIAS_MEAN + 0.5 * jax.random.normal(ks[9], (DEPTH, N_HEADS_B), f32),
        'rel_bias': 0.5 * jax.random.normal(ks[10], (N_BUCKETS, N_HEADS_A), f32),
        'w_up_a': dense(ks[11], (DEPTH, WA, D_MODEL), WA),
        'w_up_b': dense(ks[12], (DEPTH, WB, D_MODEL), WB),
        'w_o': dense(ks[13], (DEPTH, D_MODEL, D_MODEL), D_MODEL),
        'g_ffn2': gain(ks[14], (DEPTH, D_MODEL)),
        'ffn2_w_in': dense(ks[15], (DEPTH, D_MODEL, 2 * D_FF), D_MODEL),
        'ffn2_w_out': dense(ks[16], (DEPTH, D_FF, D_MODEL), D_FF),
        'g_final': gain(ks[17], (D_MODEL,)),
    }


def reference(x, c, w_ada, b_ada, g_ffn1, ffn1_w_in, ffn1_w_out, g_mix, w_in, b_forget,
              rel_bias, w_up_a, w_up_b, w_o, g_ffn2, ffn2_w_in, ffn2_w_out, g_final):
    c_act = jax.nn.silu(c)
    for l in range(DEPTH):
        mod = c_act @ w_ada[l] + b_ada[l]
        sh1, sc1, gt1, sh2, sc2, gt2, sh3, sc3, gt3 = jnp.split(mod, N_MOD, axis=-1)
        h = modulate(rms_norm(x, g_ffn1[l]), sh1, sc1)
        x = x + 0.5 * gt1[:, None, :] * swiglu(h, ffn1_w_in[l], ffn1_w_out[l])
        h = modulate(rms_norm(x, g_mix[l]), sh2, sc2)
        x = x + gt2[:, None, :] * hybrid_mixer(h, w_in[l], b_forget[l], rel_bias,
                                               w_up_a[l], w_up_b[l], w_o[l])
        h = modulate(rms_norm(x, g_ffn2[l]), sh3, sc3)
        x = x + 0.5 * gt3[:, None, :] * swiglu(h, ffn2_w_in[l], ffn2_w_out[l])
    return rms_norm(x, g_final)
```

```python
import os
import math
import contextlib
import numpy as np
import concourse.bass as bass
import concourse.mybir as mybir
from concourse.bass_utils import run_bass_kernel_spmd

F32 = mybir.dt.float32
BF16 = mybir.dt.bfloat16
AF = mybir.ActivationFunctionType
ALU = mybir.AluOpType
AX = mybir.AxisListType

D = 4096
DFF = 11008
SEQ = 2048
NB = 16
QB = 128
HD = 128
NH = 16
DIN = 17760
EPS = 1e-6
NEG = -30000.0
NEGF = -1.0e30
STAGE = int(os.environ.get("MK_STAGE", "99"))
DEBUG = [v for v in os.environ.get("MK_DEBUG", "").split(",") if v]
TEST = os.environ.get("MK_TEST", "")
DECLARED = []

O_QA, O_KA, O_VA, O_QI, O_KI, O_WI, O_QB, O_KB, O_VB, O_FB, O_GA, O_GB = (
    0, 2048, 2176, 2304, 3328, 3392, 3408, 5456, 7504, 9552, 9568, 13664)

OWN = {0: [0, 3, 4, 7, 8, 11, 12, 15], 1: [1, 2, 5, 6, 9, 10, 13, 14]}


class Prog:
    ENG = ("sync", "scalar", "vector", "gpsimd", "tensor")

    def __init__(self, nc):
        self.nc = nc
        self.q = {k: [] for k in self.ENG}
        self.cnt = {}
        self.waited = {k: {} for k in self.ENG}
        self.sems = {}
        self.pending = {k: [] for k in self.ENG}
        self.last = {}

    def sem(self, name):
        if name not in self.sems:
            self.sems[name] = self.nc.alloc_semaphore(name=name)
            self.cnt[name] = 0
        return name

    def op(self, eng, fn, waits=(), inc=None, amt=1):
        ws = []
        for w in waits:
            if w is None:
                continue
            s, v = w
            if self.waited[eng].get(s, 0) >= v:
                continue
            self.waited[eng][s] = v
            ws.append((self.sems[s], v))
        tok = None
        semh = None
        if inc is not None:
            self.sem(inc)
            self.cnt[inc] += amt
            tok = (inc, self.cnt[inc])
            semh = self.sems[inc]
            if amt == 1:
                self.last[eng] = tok

        def run(e, ws=ws, fn=fn, semh=semh, amt=amt):
            for (sh, v) in ws:
                e.wait_ge(sh, v)
            ins = fn(e)
            if semh is not None:
                ins.then_inc(semh, amt)
        self.q[eng].append(run)
        return tok

    def dma(self, eng, out, in_, waits=(), inc=None, **kw):
        tok = self.op(eng, lambda e: e.dma_start(out=out, in_=in_, **kw), waits, inc, 16)
        if tok is not None:
            self.pending[eng].append(tok)
        return tok

    def barrier(self):
        best = {}
        for e in self.ENG:
            for tok in self.pending[e]:
                if tok is not None and best.get(tok[0], 0) < tok[1]:
                    best[tok[0]] = tok[1]
            self.pending[e] = []
            tok = self.last.get(e)
            if tok is not None and best.get(tok[0], 0) < tok[1]:
                best[tok[0]] = tok[1]
        toks = list(best.items())
        for e in self.ENG:
            self.op(e, lambda en: en.nop(), toks)

    def emit(self):
        with self.nc.Block() as block:
            for name in self.ENG:
                def body(e, name=name):
                    for f in self.q[name]:
                        f(e)
                getattr(block, name)(body)


def build_program():
    nc = bass.Bass("TRN2", target_bir_lowering=False)
    P = Prog(nc)

    def din(name, shape):
        DECLARED.append(name)
        return nc.dram_tensor(name, list(shape), F32, kind="ExternalInput").ap()

    def scratch(name, shape, dt):
        if name in DEBUG:
            return nc.dram_tensor(name, list(shape), dt, kind="ExternalOutput").ap()
        return nc.dram_tensor(name, list(shape), dt).ap()

    x_in = din("x", [SEQ, D])
    c_fm = din("c_fm", [128, 32])
    if not TEST:
        w_ada = din("w_ada", [D, 9 * D])
        b_ada = din("b_ada", [1, 9 * D])
    g1_fm = din("g1_fm", [128, 32])
    g2_fm = din("g2_fm", [128, 32])
    g3_fm = din("g3_fm", [128, 32])
    g_fin = din("g_fin", [1, D])
    if TEST in ("", "ffn"):
        f1_wi = din("f1_wi", [D, 2 * DFF])
        f1_wo = din("f1_wo", [DFF, D])
    if TEST in ("",):
        f2_wi = din("f2_wi", [D, 2 * DFF])
        f2_wo = din("f2_wo", [DFF, D])
    if TEST in ("", "mix"):
        w_in = din("w_in", [D, DIN])
        w_upa = din("w_upa", [2048, D])
        w_upb = din("w_upb", [2048, D])
        w_o = din("w_o", [D, D])
    b_fg = din("b_fg", [1, NH])
    rel_b = din("rel_b", [32, NH])
    cst = din("cst", [128, 512])
    pmk = din("pmk", [128, 512])
    Lmat = din("Lmat", [SEQ, SEQ])
    oh = din("oh", [6, 33, 128 * 128])
    out = nc.dram_tensor("out", [1024, D], F32, kind="ExternalOutput").ap()

    modbc = din("modbc_in", [128, 9 * D]) if TEST else scratch("modbc", [128, 9 * D], F32)
    x1d = scratch("x1d", [SEQ, D], F32)
    x2d = scratch("x2d", [1024, D], F32)
    x3d = scratch("x3d", [1024, D], F32)
    qaT = scratch("qaT", [128, NH, 1024], BF16)
    kaT = scratch("kaT", [128, SEQ], BF16)
    va = scratch("va", [SEQ, 128], BF16)
    qiT = scratch("qiT", [128, 8, 1024], BF16)
    kiT = scratch("kiT", [64, SEQ], BF16)
    wi = scratch("wi", [1024, NH], F32)
    qbT = scratch("qbT", [128, NH, 1024], BF16)
    kbT = scratch("kbT", [128, NH, SEQ], BF16)
    vb = scratch("vb", [SEQ, NH * HD], BF16)
    fb = scratch("fb", [SEQ, NH], F32)
    gaT = scratch("gaT", [128, 32, 1024], BF16)
    gbT = scratch("gbT", [128, 32, 1024], BF16)
    FTd = scratch("FTd", [NH, SEQ], F32)
    nFTd = scratch("nFTd", [NH, SEQ], F32)
    btd = scratch("btd", [6, NH, 128, 128], F32)
    oaT = scratch("oaT", [128, NH, 1024], BF16)
    obT = scratch("obT", [128, NH, 1024], BF16)

    ps = [nc.alloc_psum_tensor(f"ps{i}", [128, 512], F32) for i in range(8)]
    psfree = [None] * 8

    cf = nc.alloc_sbuf_tensor("cf", [128, 512], F32)
    cb = nc.alloc_sbuf_tensor("cb", [128, 512], BF16)
    pmf = nc.alloc_sbuf_tensor("pmf", [128, 512], F32)
    pmb = nc.alloc_sbuf_tensor("pmb", [128, 256], BF16)
    modT = nc.alloc_sbuf_tensor("modT", [128, 9, 32], F32)
    AB = nc.alloc_sbuf_tensor("AB", [128, 6, 32], F32)
    ident_f = cf[:, 0:128]
    ident_b = cb[:, 0:128]
    tri_st_b = cb[:, 128:256]
    tri_ts_f = cf[:, 256:384]
    ones_b = cb[:, 384:512]

    t_c1 = P.dma("sync", cf[:, :], cst, inc="cld_a")
    t_c2 = P.dma("gpsimd", cb[:, :], cst, inc="cld_b")
    t_c3 = P.dma("sync", pmf[:, :], pmk, inc="cld_c")
    t_c4 = P.dma("gpsimd", pmb[:, :], pmk[:, 0:256], inc="cld_d")
    TC = [t_c1, t_c2, t_c3, t_c4]

    NSL = 4
    KS = 8
    wr = [nc.alloc_sbuf_tensor(f"wr{i}", [128, KS, 512], BF16) for i in range(NSL)]
    wr_free = [None] * NSL
    wr_n = [0]

    def wload(dram2d, kc, ncols):
        s = wr_n[0] % NSL
        wr_n[0] += 1
        view = wr[s][:, 0:kc, 0:ncols]
        tok = P.dma("gpsimd", view, dram2d.rearrange("(kc p) n -> p kc n", p=128),
                    waits=[wr_free[s]], inc=f"wl{s}")
        return s, tok

    def wrelease(s, tok):
        wr_free[s] = tok

    if TEST:
        AB_in = din("AB_in", [128, 6 * 32])
        t_ab = P.dma("sync", AB[:, :, :], AB_in.rearrange("p (a b) -> p a b", b=32), inc="abld")
        P.barrier()
    with contextlib.ExitStack() as es:
      if not TEST:
          def sb(name, shape, dt):
              return es.enter_context(nc.sbuf_tensor(name, list(shape), dt))
          cfm = sb("cfm", [128, 32], F32)
          cact = sb("cact", [128, 32], F32)
          cbc = sb("cbc", [128, 32, 128], BF16)
          wm = [sb(f"wm{i}", [128, 32, 512], BF16) for i in range(2)]
          bab = [sb(f"bab{i}", [128, 512], F32) for i in range(2)]
          mblk = [sb(f"mblk{i}", [128, 512], F32) for i in range(2)]
          dtmp = sb("dtmp", [128, 4, 128], F32)
          gfm = sb("gfm", [128, 3, 32], F32)

          t = P.dma("sync", cfm[:, :], c_fm, inc="m_ld")
          t = P.op("scalar", lambda e: e.activation(out=cact[:, :], in_=cfm[:, :], func=AF.Silu), [t], inc="act")
          t_cbc = P.op("vector", lambda e: e.tensor_copy(out=cbc[:, :, :],
                                                          in_=cact[:, :].unsqueeze(2).broadcast_to([128, 32, 128])),
                       [t], inc="dve")
          tg = P.dma("sync", gfm[:, 0, :], g1_fm, inc="m_ldg")
          tg = P.dma("sync", gfm[:, 1, :], g2_fm, inc="m_ldg")
          tg = P.dma("sync", gfm[:, 2, :], g3_fm, inc="m_ldg")
          wm_free = [None, None]
          bab_free = [None, None]
          mblk_free = [None, None]
          last_dve = None
          for blk in range(72):
              s = blk % 2
              c0 = blk * 512
              tw = P.dma("gpsimd", wm[s][:, :, :], w_ada[:, c0:c0 + 512].rearrange("(kc p) n -> p kc n", p=128),
                         waits=[wm_free[s]], inc=f"wm{s}")
              tb = P.dma("sync", bab[s][:, :], b_ada[0:1, c0:c0 + 512].broadcast_to([128, 512]),
                         waits=[bab_free[s]], inc=f"bab{s}")
              tm = None
              for kc in range(32):
                  tm = P.op("tensor", lambda e, s=s, kc=kc: e.matmul(ps[s][:, :], lhsT=cbc[:, kc, :], rhs=wm[s][:, kc, :],
                                                                     start=(kc == 0), stop=(kc == 31)),
                            [tw, t_cbc, psfree[s]], inc="pe" if kc == 31 else None)
              wm_free[s] = tm
              te = P.op("vector", lambda e, s=s: e.tensor_tensor(out=mblk[s][:, :], in0=ps[s][:, :], in1=bab[s][:, :], op=ALU.add),
                        [tm, tb, mblk_free[s], last_dve], inc="dve")
              psfree[s] = te
              bab_free[s] = te
              v = blk // 8
              if v % 3 != 2:
                  ch0 = (blk % 8) * 4
                  t1 = P.op("vector", lambda e, s=s: e.tensor_tensor(
                      out=dtmp[:, :, :], in0=mblk[s][:, :].rearrange("p (a b) -> p a b", b=128),
                      in1=ident_f.unsqueeze(1).broadcast_to([128, 4, 128]), op=ALU.mult), [te, TC[0]], inc="dve")
                  te2 = P.op("vector", lambda e, v=v, ch0=ch0: e.tensor_reduce(
                      out=modT[:, v, ch0:ch0 + 4], in_=dtmp[:, :, :], axis=AX.X, op=ALU.add), [t1], inc="dve")
                  last_dve = te2
              else:
                  last_dve = te
              tst = P.dma("sync", modbc[:, c0:c0 + 512], mblk[s][:, :], [last_dve], inc=f"mst{s}")
              mblk_free[s] = tst
          for k in range(3):
              t1 = P.op("vector", lambda e, k=k: e.tensor_scalar(out=AB[:, 2 * k, :], in0=modT[:, 3 * k + 1, :],
                                                                 scalar1=1.0, scalar2=None, op0=ALU.add),
                        [last_dve, tg], inc="dve")
              t1 = P.op("vector", lambda e, k=k: e.tensor_tensor(out=AB[:, 2 * k, :], in0=AB[:, 2 * k, :], in1=gfm[:, k, :], op=ALU.mult),
                        [t1], inc="dve")
              last_dve = P.op("vector", lambda e, k=k: e.tensor_copy(out=AB[:, 2 * k + 1, :], in_=modT[:, 3 * k, :]), [t1], inc="dve")
          P.barrier()
    if STAGE <= 0:
        return finish(nc, P, out)

    def norm_tile(es, xsrc, row0, k, hT, tagp):
        xb = es["xb"]
        xn = es["xn"]
        ss = es["ss"]
        last = None
        for b in range(4):
            r0 = row0 + b * 128
            tl = P.dma("sync", xb[:, :], xsrc[r0:r0 + 128, :], waits=[es.get("xb_free")], inc="xld")
            t1 = P.op("scalar", lambda e: e.activation(out=xn[:, :], in_=xb[:, :], func=AF.Square),
                      [tl, es.get("xn_free")], inc="act")
            t1 = P.op("vector", lambda e: e.reduce_sum(out=ss[:, 0:1], in_=xn[:, :], axis=AX.X), [t1, es.get("ss_free")], inc="dve")
            t2 = P.op("vector", lambda e: e.tensor_scalar(out=ss[:, 1:2], in0=ss[:, 0:1], scalar1=1.0 / D, scalar2=EPS,
                                                          op0=ALU.mult, op1=ALU.add), [t1], inc="dve")
            t2 = P.op("scalar", lambda e: e.activation(out=ss[:, 2:3], in_=ss[:, 1:2], func=AF.Sqrt), [t2], inc="act")
            t2 = P.op("vector", lambda e: e.reciprocal(out=ss[:, 3:4], in_=ss[:, 2:3]), [t2], inc="dve")
            t3 = P.op("scalar", lambda e: e.activation(out=xn[:, :], in_=xb[:, :], func=AF.Identity, scale=ss[:, 3:4]),
                      [t2], inc="act")
            es["xb_free"] = t3
            es["ss_free"] = t3
            tlast = None
            for g in range(4):
                bank = 4 + (g % 2)
                pv = ps[bank][:, :].bitcast(BF16)
                tt = None
                for c in range(8):
                    ch = g * 8 + c
                    tt = P.op("tensor", lambda e, pv=pv, c=c, ch=ch: e.transpose(
                        out=pv[:, c * 128:(c + 1) * 128], in_=xn[:, ch * 128:(ch + 1) * 128], identity=ident_b),
                        [t3, psfree[bank], TC[1]], inc="pe" if c == 7 else None)
                ta_last = None
                tv_last = None
                for c in range(8):
                    ch = g * 8 + c
                    if g % 2 == 0:
                        tv_last = P.op("vector", lambda e, pv=pv, c=c, ch=ch, b=b: e.tensor_scalar(
                            out=hT[:, ch, b * 128:(b + 1) * 128], in0=pv[:, c * 128:(c + 1) * 128],
                            scalar1=AB[:, 2 * k, ch:ch + 1], scalar2=AB[:, 2 * k + 1, ch:ch + 1],
                            op0=ALU.mult, op1=ALU.add), [tt, es.get("hT_free")], inc="dve")
                    else:
                        ta_last = P.op("scalar", lambda e, pv=pv, c=c, ch=ch, b=b: e.activation(
                            out=hT[:, ch, b * 128:(b + 1) * 128], in_=pv[:, c * 128:(c + 1) * 128],
                            func=AF.Identity, scale=AB[:, 2 * k, ch:ch + 1], bias=AB[:, 2 * k + 1, ch:ch + 1]),
                            [tt, es.get("hT_free")], inc="act")
                psfree[bank] = tv_last if g % 2 == 0 else ta_last
                if g == 2:
                    last_v = tv_last
                if g == 3:
                    last = P.op("vector", lambda e: e.nop(), [ta_last, last_v])
                    last = P.op("vector", lambda e: e.memset(ss[:, 0:1], 0.0), [ta_last, last_v], inc="dve")
            es["xn_free"] = last
        return last

    def ffn(xsrc, xdst, ntiles, k, wi_d, wo_d, gate_v, half):
        with contextlib.ExitStack() as st:
            def sb(name, shape, dt):
                return st.enter_context(nc.sbuf_tensor(name, list(shape), dt))
            hT = sb(f"f{k}_hT", [128, 32, 512], BF16)
            uT = sb(f"f{k}_uT", [128, 86, 512], BF16)
            es = {"xb": sb(f"f{k}_xb", [128, D], F32), "xn": sb(f"f{k}_xn", [128, D], BF16),
                  "ss": sb(f"f{k}_ss", [128, 4], F32)}
            sl = [sb(f"f{k}_sl{i}", [128, 512], F32) for i in range(2)]
            gt = [sb(f"f{k}_gt{i}", [128, 512], F32) for i in range(2)]
            xt = [sb(f"f{k}_xt{i}", [128, 512], F32) for i in range(4)]
            yo = [sb(f"f{k}_yo{i}", [128, 512], F32) for i in range(4)]
            sl_free = [None, None]
            gt_free = [None, None]
            xt_free = [None] * 4
            yo_free = [None] * 4
            nsl = 0
            nep = 0
            u_done = None
            for tt in range(ntiles):
                row0 = tt * 512
                es["hT_free"] = u_done
                th = norm_tile(es, xsrc, row0, k, hT, "f")
                if "n" == os.environ.get("MK_FFN_PARTS", ""):
                    continue
                pa_last = None
                for fb_ in range(43):
                    f0 = fb_ * 256
                    banks = [0, 1, 2, 3] if fb_ % 2 == 0 else [4, 5, 6, 7]
                    for hf in range(4):
                        s, tw = wload(wi_d[hf * 1024:(hf + 1) * 1024, f0:f0 + 256], KS, 256)
                        tw2 = P.dma("gpsimd", wr[s][:, 0:KS, 256:512],
                                    wi_d[hf * 1024:(hf + 1) * 1024, DFF + f0:DFF + f0 + 256].rearrange("(kc p) n -> p kc n", p=128),
                                    waits=[wr_free[s]], inc=f"wl{s}")
                        tm = None
                        for q in range(4):
                            for kc in range(KS):
                                first = (hf == 0 and kc == 0)
                                lastk = (hf == 3 and kc == KS - 1)
                                tm = P.op("tensor", lambda e, s=s, q=q, kc=kc, hf=hf, first=first, lastk=lastk, bk=banks[q]: e.matmul(
                                    ps[bk][:, :], lhsT=wr[s][:, kc, q * 128:(q + 1) * 128], rhs=hT[:, hf * KS + kc, :],
                                    start=first, stop=lastk),
                                    [tw2, tw, th, psfree[banks[q]] if first else None],
                                    inc="pe" if (kc == KS - 1) else None)
                        wrelease(s, tm)
                    pa_last = tm
                    for q in range(2):
                        ssl = nsl % 2
                        nsl += 1
                        ta = P.op("scalar", lambda e, ssl=ssl, bk=banks[q]: e.activation(out=sl[ssl][:, :], in_=ps[bk][:, :], func=AF.Silu),
                                  [tm, sl_free[ssl]], inc="act")
                        tv = P.op("vector", lambda e, ssl=ssl, bk=banks[2 + q], fc=fb_ * 2 + q: e.tensor_tensor(
                            out=uT[:, fc, :], in0=ps[bk][:, :], in1=sl[ssl][:, :], op=ALU.mult),
                            [ta, es.get("uT_free")], inc="dve")
                        sl_free[ssl] = tv
                        psfree[banks[q]] = ta
                        psfree[banks[2 + q]] = tv
                u_ready = tv
                u_done = pa_last
                if "na" == os.environ.get("MK_FFN_PARTS", ""):
                    continue
                pb_last = None
                for ct in range(8):
                    c0 = ct * 512
                    banks = [0, 1, 2, 3] if ct % 2 == 0 else [4, 5, 6, 7]
                    gs = ct % 2
                    tg_ = P.dma("sync", gt[gs][:, :], modbc[:, gate_v * D + c0:gate_v * D + c0 + 512],
                                waits=[gt_free[gs]], inc=f"gld{gs}")
                    tm = None
                    for pc in range(11):
                        fc0 = pc * KS
                        nfc = min(KS, 86 - fc0)
                        s, tw = wload(wo_d[fc0 * 128:(fc0 + nfc) * 128, c0:c0 + 512], nfc, 512)
                        for tb in range(4):
                            for i in range(nfc):
                                fc = fc0 + i
                                tm = P.op("tensor", lambda e, s=s, i=i, fc=fc, tb=tb, bk=banks[tb]: e.matmul(
                                    ps[bk][:, :], lhsT=uT[:, fc, tb * 128:(tb + 1) * 128], rhs=wr[s][:, i, :],
                                    start=(fc == 0), stop=(fc == 85)),
                                    [tw, u_ready, psfree[banks[tb]] if fc == 0 else None],
                                    inc="pe" if i == nfc - 1 else None)
                        wrelease(s, tm)
                    pb_last = tm
                    for tb in range(4):
                        se = nep % 4
                        nep += 1
                        r0 = row0 + tb * 128
                        tx = P.dma("sync", xt[se][:, :], xsrc[r0:r0 + 128, c0:c0 + 512], waits=[xt_free[se]], inc=f"xtl{se}")
                        t1 = P.op("vector", lambda e, se=se, bk=banks[tb], gs=gs: e.scalar_tensor_tensor(
                            out=yo[se][:, :], in0=ps[bk][:, :], scalar=half, in1=gt[gs][:, :], op0=ALU.mult, op1=ALU.mult),
                            [tm, tg_, yo_free[se]], inc="dve")
                        psfree[banks[tb]] = t1
                        t2 = P.op("vector", lambda e, se=se: e.tensor_tensor(out=yo[se][:, :], in0=yo[se][:, :], in1=xt[se][:, :], op=ALU.add),
                                  [t1, tx], inc="dve")
                        xt_free[se] = t2
                        ts = P.dma("sync", xdst[r0:r0 + 128, c0:c0 + 512], yo[se][:, :], [t2], inc=f"yst{se}")
                        yo_free[se] = ts
                    gt_free[gs] = t1
                es["uT_free"] = pb_last
            P.barrier()

    if TEST in ("", "ffn"):
        ffn(x_in, x1d, 1 if TEST else 4, 0, f1_wi, f1_wo, 2, 0.5)
    if STAGE <= 1:
        return finish(nc, P, out, copy_from=None)

    SC = HD ** -0.5
    x1src = x_in if TEST == "mix" else x1d

    def mixer_proj():
        with contextlib.ExitStack() as st:
            def sb(name, shape, dt):
                return st.enter_context(nc.sbuf_tensor(name, list(shape), dt))
            hT = sb("m_hT", [128, 32, 512], BF16)
            es = {"xb": sb("m_xb", [128, D], F32), "xn": sb("m_xn", [128, D], BF16), "ss": sb("m_ss", [128, 4], F32)}
            stg = [sb(f"m_stg{i}", [128, 4, 512], BF16) for i in range(2)]
            stg_free = [None, None]
            stt = [sb(f"m_stt{i}", [128, 512], BF16) for i in range(4)]
            stt_free = [None] * 4
            stf = [sb(f"m_stf{i}", [128, 16], F32) for i in range(4)]
            stf_free = [None] * 4
            cn = {"stg": 0, "stt": 0, "stf": 0, "pc": 0}
            lastmm = [None]

            def fm_piece(col0, ncols, dest, func, scale, th):
                nq = (ncols + 127) // 128
                w = min(128, ncols)
                banks = [0, 1, 2, 3] if cn["pc"] % 2 == 0 else [4, 5, 6, 7]
                cn["pc"] += 1
                tm = None
                for hf in range(4):
                    s, tw = wload(w_in[hf * 1024:(hf + 1) * 1024, col0:col0 + ncols], KS, ncols)
                    for q in range(nq):
                        for kc in range(KS):
                            first = (hf == 0 and kc == 0)
                            lastk = (hf == 3 and kc == KS - 1)
                            tm = P.op("tensor", lambda e, s=s, q=q, kc=kc, hf=hf, first=first, lastk=lastk, bk=banks[q]: e.matmul(
                                ps[bk][0:w, :], lhsT=wr[s][:, kc, q * 128:q * 128 + w], rhs=hT[:, hf * KS + kc, :],
                                start=first, stop=lastk), [tw, th, psfree[banks[q]] if first else None],
                                inc="pe" if kc == KS - 1 else None)
                    wrelease(s, tm)
                lastmm[0] = tm
                i = cn["stg"] % 2
                cn["stg"] += 1
                te = None
                for q in range(nq):
                    te = P.op("scalar", lambda e, i=i, q=q, bk=banks[q]: e.activation(
                        out=stg[i][0:w, q, :], in_=ps[bk][0:w, :], func=func, scale=scale), [tm, stg_free[i]], inc="act")
                    psfree[banks[q]] = te
                stg_free[i] = P.dma("sync", dest, stg[i][0:w, 0:nq, :], [te], inc=f"stgd{i}")

            def tm_piece(col0, ncols, dest_fn, isf32, th):
                banks = [0, 1, 2, 3] if cn["pc"] % 2 == 0 else [4, 5, 6, 7]
                cn["pc"] += 1
                tm = None
                for hf in range(4):
                    s, tw = wload(w_in[hf * 1024:(hf + 1) * 1024, col0:col0 + ncols], KS, ncols)
                    for tb in range(4):
                        for kc in range(KS):
                            first = (hf == 0 and kc == 0)
                            lastk = (hf == 3 and kc == KS - 1)
                            tm = P.op("tensor", lambda e, s=s, tb=tb, kc=kc, hf=hf, first=first, lastk=lastk, bk=banks[tb]: e.matmul(
                                ps[bk][:, 0:ncols], lhsT=hT[:, hf * KS + kc, tb * 128:(tb + 1) * 128], rhs=wr[s][:, kc, 0:ncols],
                                start=first, stop=lastk), [tw, th, psfree[banks[tb]] if first else None],
                                inc="pe" if kc == KS - 1 else None)
                    wrelease(s, tm)
                lastmm[0] = tm
                for tb in range(4):
                    if isf32:
                        i = cn["stf"] % 4
                        cn["stf"] += 1
                        te = P.op("vector", lambda e, i=i, bk=banks[tb]: e.tensor_copy(out=stf[i][:, 0:ncols], in_=ps[bk][:, 0:ncols]),
                                  [tm, stf_free[i]], inc="dve")
                        psfree[banks[tb]] = te
                        stf_free[i] = P.dma("sync", dest_fn(tb), stf[i][:, 0:ncols], [te], inc=f"stfd{i}")
                    else:
                        i = cn["stt"] % 4
                        cn["stt"] += 1
                        te = P.op("scalar", lambda e, i=i, bk=banks[tb]: e.activation(out=stt[i][:, 0:ncols], in_=ps[bk][:, 0:ncols], func=AF.Identity),
                                  [tm, stt_free[i]], inc="act")
                        psfree[banks[tb]] = te
                        stt_free[i] = P.dma("sync", dest_fn(tb), stt[i][:, 0:ncols], [te], inc=f"sttd{i}")

            for tt in range(4):
                own = tt < 2
                t0 = tt * 512
                es["hT_free"] = lastmm[0]
                th = norm_tile(es, x1src, t0, 1, hT, "m")
                fm_piece(O_KA, 128, kaT[:, t0:t0 + 512].unsqueeze(1), AF.Identity, 1.0, th)
                fm_piece(O_KI, 64, kiT[:, t0:t0 + 512].unsqueeze(1), AF.Identity, 1.0, th)
                for pc in range(4):
                    fm_piece(O_KB + pc * 512, 512, kbT[:, pc * 4:(pc + 1) * 4, t0:t0 + 512], AF.Identity, 1.0, th)
                tm_piece(O_VA, 128, lambda tb: va[t0 + tb * 128:t0 + (tb + 1) * 128, :], False, th)
                for pc in range(4):
                    tm_piece(O_VB + pc * 512, 512, lambda tb, pc=pc: vb[t0 + tb * 128:t0 + (tb + 1) * 128, pc * 512:(pc + 1) * 512], False, th)
                tm_piece(O_FB, 16, lambda tb: fb[t0 + tb * 128:t0 + (tb + 1) * 128, :], True, th)
                if own:
                    for pc in range(4):
                        fm_piece(O_QA + pc * 512, 512, qaT[:, pc * 4:(pc + 1) * 4, t0:t0 + 512], AF.Identity, SC, th)
                    for pc in range(2):
                        fm_piece(O_QI + pc * 512, 512, qiT[:, pc * 4:(pc + 1) * 4, t0:t0 + 512], AF.Identity, 1.0, th)
                    tm_piece(O_WI, 16, lambda tb: wi[t0 + tb * 128:t0 + (tb + 1) * 128, :], True, th)
                    for pc in range(4):
                        fm_piece(O_QB + pc * 512, 512, qbT[:, pc * 4:(pc + 1) * 4, t0:t0 + 512], AF.Identity, SC, th)
                    for pc in range(8):
                        fm_piece(O_GA + pc * 512, 512, gaT[:, pc * 4:(pc + 1) * 4, t0:t0 + 512], AF.Sigmoid, 1.0, th)
                    for pc in range(8):
                        fm_piece(O_GB + pc * 512, 512, gbT[:, pc * 4:(pc + 1) * 4, t0:t0 + 512], AF.Sigmoid, 1.0, th)
            P.barrier()

    def fcumsum():
        with contextlib.ExitStack() as st:
            def sb(name, shape, dt):
                return st.enter_context(nc.sbuf_tensor(name, list(shape), dt))
            fbs = sb("c_fb", [128, 16, 16], F32)
            bfg = sb("c_bfg", [128, 16], F32)
            lf = sb("c_lf", [128, 16, 16], F32)
            Lb = [sb(f"c_L{i}", [128, 4, 512], F32) for i in range(2)]
            L_free = [None, None]
            FT = sb("c_FT", [16, 2048], F32)
            nFT = sb("c_nFT", [16, 2048], F32)
            t1 = P.dma("sync", fbs[:, :, :], fb.rearrange("(b p) h -> p b h", p=128), inc="c_ld1")
            t2 = P.dma("sync", bfg[:, :], b_fg[0:1, :].broadcast_to([128, 16]), inc="c_ld2")
            ta = P.op("vector", lambda e: e.tensor_tensor(out=lf[:, :, :], in0=fbs[:, :, :],
                                                          in1=bfg[:, :].unsqueeze(1).broadcast_to([128, 16, 16]), op=ALU.add),
                      [t1, t2], inc="dve")
            ta = P.op("scalar", lambda e: e.activation(out=lf[:, :, :], in_=lf[:, :, :], func=AF.Exp, scale=-1.0), [ta], inc="act")
            ta = P.op("scalar", lambda e: e.activation(out=lf[:, :, :], in_=lf[:, :, :], func=AF.Ln, bias=1.0), [ta], inc="act")
            ta = P.op("vector", lambda e: e.tensor_scalar(out=lf[:, :, :], in0=lf[:, :, :], scalar1=-1.0, scalar2=None, op0=ALU.mult),
                      [ta], inc="dve")
            n = 0
            for tc in range(4):
                tm = None
                for bg in range(4):
                    i = n % 2
                    n += 1
                    tl = P.dma("sync", Lb[i][:, :, :],
                               Lmat[bg * 512:(bg + 1) * 512, tc * 512:(tc + 1) * 512].rearrange("(b p) t -> p b t", p=128),
                               waits=[L_free[i]], inc=f"c_L{i}")
                    for bb in range(4):
                        bs = bg * 4 + bb
                        tm = P.op("tensor", lambda e, i=i, bb=bb, bs=bs, tc=tc: e.matmul(
                            ps[tc][0:16, :], lhsT=lf[:, bs, :], rhs=Lb[i][:, bb, :], start=(bs == 0), stop=(bs == 15)),
                            [tl, ta, psfree[tc] if bs == 0 else None], inc="pe" if bb == 3 else None)
                    L_free[i] = tm
                te = P.op("scalar", lambda e, tc=tc: e.activation(out=FT[:, tc * 512:(tc + 1) * 512], in_=ps[tc][0:16, :], func=AF.Identity),
                          [tm], inc="act")
                te2 = P.op("vector", lambda e, tc=tc: e.tensor_scalar(out=nFT[:, tc * 512:(tc + 1) * 512], in0=ps[tc][0:16, :],
                                                                     scalar1=-1.0, scalar2=None, op0=ALU.mult), [tm, te], inc="dve")
                psfree[tc] = te2
            P.dma("sync", FTd, FT[:, :], [te2], inc="c_st1")
            P.dma("sync", nFTd, nFT[:, :], [te2], inc="c_st2")
            P.barrier()

    def t5tables(BT):
        with contextlib.ExitStack() as st:
            def sb(name, shape, dt):
                return st.enter_context(nc.sbuf_tensor(name, list(shape), dt))
            rb = sb("t_rb", [33, 16], F32)
            rb31 = sb("t_rb31", [32, 16], F32)
            ohb = [sb(f"t_oh{i}", [33, 4096], F32) for i in range(2)]
            oh_free = [None, None]
            st5 = [sb(f"t_st{i}", [16, 512], F32) for i in range(2)]
            st_free = [None, None]
            t1 = P.dma("sync", rb[0:32, :], rel_b, inc="t_ld1")
            t2 = P.dma("sync", rb31[:, :], rel_b[31:32, :].broadcast_to([32, 16]), inc="t_ld2")
            t3 = P.op("vector", lambda e: e.memset(rb[32:33, :], NEG), [], inc="dve")
            t4 = P.op("vector", lambda e: e.tensor_tensor(out=rb[0:32, :], in0=rb[0:32, :], in1=rb31[:, :], op=ALU.subtract),
                      [t1, t2, t3], inc="dve")
            n = 0
            m = 0
            for slot in range(6):
                flat = btd[slot].rearrange("h s t -> h (s t)")
                for pc in range(4):
                    i = n % 2
                    n += 1
                    tl = P.dma("sync", ohb[i][:, :], oh[slot, :, pc * 4096:(pc + 1) * 4096], waits=[oh_free[i]], inc=f"t_oh{i}")
                    tm = None
                    for c in range(8):
                        bk = m % 4
                        j = m % 2
                        m += 1
                        tm = P.op("tensor", lambda e, i=i, c=c, bk=bk: e.matmul(
                            ps[bk][0:16, :], lhsT=rb[:, :], rhs=ohb[i][:, c * 512:(c + 1) * 512], start=True, stop=True),
                            [tl, t4, psfree[bk]], inc="pe")
                        te = P.op("scalar", lambda e, j=j, bk=bk: e.activation(out=st5[j][:, :], in_=ps[bk][0:16, :], func=AF.Identity),
                                  [tm, st_free[j]], inc="act")
                        psfree[bk] = te
                        off = pc * 4096 + c * 512
                        st_free[j] = P.dma("sync", flat[:, off:off + 512], st5[j][:, :], [te], inc=f"t_st{j}")
                    oh_free[i] = tm
            P.barrier()
            for slot in range(6):
                P.dma("gpsimd", BT[:, slot, :, :], btd[slot].rearrange("h s t -> s h t"), inc="t_bt")
            P.barrier()

    ablk = [sum(2 * (jj + 1) for jj in range(j)) for j in range(8)]

    def indexer(A_all):
        with contextlib.ExitStack() as st:
            def sb(name, shape, dt):
                return st.enter_context(nc.sbuf_tensor(name, list(shape), dt))
            qi_sb = sb("i_qi", [128, 8, 1024], BF16)
            ki2 = sb("i_ki", [128, 2048], BF16)
            wi_sb = sb("i_wi", [128, 8, 16], F32)
            acc = sb("i_acc", [128, 2048], F32)
            wk = sb("i_wk", [128, 2048], F32)
            tmp = [sb(f"i_tmp{i}", [128, 512], F32) for i in range(2)]
            tmp_free = [None, None]
            m8 = sb("i_m8", [128, 8], F32)
            selb = sb("i_sel", [128, 2048], BF16)
            tl1 = P.dma("sync", qi_sb[:, :, :], qiT, inc="i_ld1")
            tl2 = P.dma("sync", ki2[0:64, :], kiT, inc="i_ld2")
            tl3 = P.dma("sync", ki2[64:128, :], kiT, inc="i_ld3")
            tl4 = P.dma("sync", wi_sb[:, :, :], wi.rearrange("(j p) h -> p j h", p=128), inc="i_ld4")
            LD = [tl1, tl2, tl3, tl4]
            nb = 0
            nt = 0
            acc_tok = None
            sel_free = None
            for j in range(8):
                par = j % 2
                ncol = 2 * (j + 1) * 128
                nown = (j + 1) * 128
                chunks = []
                for (a0, k0, n) in ((0, 0, nown), (nown, 1024, nown)):
                    for c0 in range(0, n, 512):
                        chunks.append((a0 + c0, k0 + c0, min(512, n - c0)))
                for (a0, k0, n) in chunks:
                    for h in range(16):
                        bk = nb % 4
                        nb += 1
                        base = 64 * (h % 2)
                        c = h // 2
                        tm = P.op("tensor", lambda e, bk=bk, base=base, c=c, j=j, k0=k0, n=n: e.matmul(
                            ps[bk][:, 0:n], lhsT=qi_sb[base:base + 64, c, j * 128:(j + 1) * 128], rhs=ki2[base:base + 64, k0:k0 + n],
                            start=True, stop=True), LD + [psfree[bk]], inc="pe")
                        if h == 0:
                            tv = P.op("vector", lambda e, bk=bk, a0=a0, n=n, j=j: e.tensor_scalar(
                                out=acc[:, a0:a0 + n], in0=ps[bk][:, 0:n], scalar1=0.0, scalar2=wi_sb[:, j, 0:1],
                                op0=ALU.max, op1=ALU.mult), [tm, acc_tok], inc="dve")
                            psfree[bk] = tv
                            acc_tok = tv
                        else:
                            ti = nt % 2
                            nt += 1
                            tv = P.op("vector", lambda e, bk=bk, ti=ti, n=n, j=j, h=h: e.tensor_scalar(
                                out=tmp[ti][:, 0:n], in0=ps[bk][:, 0:n], scalar1=0.0, scalar2=wi_sb[:, j, h:h + 1],
                                op0=ALU.max, op1=ALU.mult), [tm, tmp_free[ti]], inc="dve")
                            psfree[bk] = tv
                            tp = P.op("gpsimd", lambda e, ti=ti, a0=a0, n=n: e.tensor_tensor(
                                out=acc[:, a0:a0 + n], in0=acc[:, a0:a0 + n], in1=tmp[ti][:, 0:n], op=ALU.add),
                                [tv, acc_tok], inc="pool")
                            tmp_free[ti] = tp
                            acc_tok = tp
                tp = P.op("gpsimd", lambda e, j=j: e.tensor_tensor(out=acc[:, j * 128:(j + 1) * 128], in0=acc[:, j * 128:(j + 1) * 128],
                                                                   in1=tri_ts_f, op=ALU.add), [acc_tok, TC[0]], inc="pool")
                pc0 = (2 * j + 1) * 128
                tp = P.op("gpsimd", lambda e, pc0=pc0, par=par: e.tensor_tensor(
                    out=acc[:, pc0:pc0 + 128], in0=acc[:, pc0:pc0 + 128], in1=pmf[:, 256 + par * 128:256 + (par + 1) * 128], op=ALU.add),
                    [tp, TC[2]], inc="pool")
                t = P.op("gpsimd", lambda e, ncol=ncol: e.tensor_copy(out=wk[:, 0:ncol], in_=acc[:, 0:ncol]), [tp, sel_free], inc="pool")
                for r in range(32):
                    t = P.op("vector", lambda e, ncol=ncol: e.max(out=m8[:, :], in_=wk[:, 0:ncol]), [t], inc="dve")
                    if r < 31:
                        t = P.op("vector", lambda e, ncol=ncol: e.match_replace(out=wk[:, 0:ncol], in_to_replace=m8[:, :],
                                                                                in_values=wk[:, 0:ncol], imm_value=-3.0e38), [t], inc="dve")
                ts = P.op("vector", lambda e, ncol=ncol: e.tensor_scalar(out=selb[:, 0:ncol], in0=acc[:, 0:ncol], scalar1=m8[:, 7:8],
                                                                         scalar2=NEG, op0=ALU.is_lt, op1=ALU.mult), [t, sel_free], inc="dve")
                acc_tok = ts
                nblk = 2 * (j + 1)
                tlast = None
                for g0 in range(0, nblk, 8):
                    ng = min(8, nblk - g0)
                    bank = 4 + ((g0 // 8) % 2)
                    pv = ps[bank][:, :].bitcast(BF16)
                    tt = None
                    for c in range(ng):
                        kb = g0 + c
                        tt = P.op("tensor", lambda e, pv=pv, c=c, kb=kb: e.transpose(
                            out=pv[:, c * 128:(c + 1) * 128], in_=selb[:, kb * 128:(kb + 1) * 128], identity=ident_b),
                            [ts, psfree[bank], TC[1]], inc="pe" if c == ng - 1 else None)
                    te = P.op("scalar", lambda e, pv=pv, ng=ng, g0=g0, j=j: e.activation(
                        out=A_all[:, ablk[j] + g0:ablk[j] + g0 + ng, :], in_=pv[:, 0:ng * 128].rearrange("p (a b) -> p a b", b=128),
                        func=AF.Identity), [tt], inc="act")
                    psfree[bank] = te
                    tlast = tt
                sel_free = tlast
            P.barrier()

    def kblist(j):
        l = []
        for jj in range(j + 1):
            l.append((jj, "od" if jj == j else ("op" if jj == j - 1 else None)))
        for jj in range(j + 1):
            l.append((8 + jj, "pd" if jj == j else None))
        return l

    def fox():
        with contextlib.ExitStack() as st:
            def sb(name, shape, dt):
                return st.enter_context(nc.sbuf_tensor(name, list(shape), dt))
            qh = [sb(f"x_q{i}", [128, 1024], BF16) for i in range(2)]
            kh = [sb(f"x_k{i}", [128, 2048], BF16) for i in range(2)]
            vh = [sb(f"x_v{i}", [128, 16, 128], BF16) for i in range(2)]
            fkl = [sb(f"x_fl{i}", [2, 2048], F32) for i in range(2)]
            fkr = [sb(f"x_fr{i}", [2, 1024], F32) for i in range(2)]
            PT = [sb(f"x_pt{i}", [128, 512], BF16) for i in range(3)]
            PT_free = [None] * 3
            of = [sb(f"x_of{i}", [128, 128], F32) for i in range(2)]
            df = [sb(f"x_df{i}", [128, 128], F32) for i in range(2)]
            odf_free = [None, None]
            ost = [sb(f"x_os{i}", [128, 1024], BF16) for i in range(2)]
            ost_free = [None, None]
            tms = None
            for i in range(2):
                tms = P.op("vector", lambda e, i=i: e.memset(fkl[i][:, :], 1.0), [], inc="dve")
                tms = P.op("vector", lambda e, i=i: e.memset(fkr[i][:, :], 1.0), [], inc="dve")
            buf_free = [tms, tms]
            npt = 0
            nbk = 0
            nn = 0
            for h in range(NH):
                i = h % 2
                w = [buf_free[i]]
                l1 = P.dma("sync", qh[i][:, :], qbT[:, h, :], waits=w, inc=f"x_l{i}")
                l2 = P.dma("sync", kh[i][:, :], kbT[:, h, :], waits=w, inc=f"x_l{i}")
                l3 = P.dma("sync", vh[i][:, :, :], vb[:, h * 128:(h + 1) * 128].rearrange("(b p) d -> p b d", p=128), waits=w, inc=f"x_l{i}")
                l4 = P.dma("sync", fkl[i][1:2, :], nFTd[h:h + 1, :], waits=w, inc=f"x_l{i}")
                l5 = P.dma("sync", fkr[i][0:1, :], FTd[h:h + 1, 0:1024], waits=w, inc=f"x_l{i}")
                LDH = [l5]
                tp = None
                tmlast = None
                for j in range(8):
                    par = j % 2
                    kl = kblist(j)
                    pob = 4 + (nn % 2)
                    pdb = 6 + (nn % 2)
                    oi = nn % 2
                    nn += 1
                    for g0 in range(0, len(kl), 4):
                        grp = kl[g0:g0 + 4]
                        ng = len(grp)
                        bank = nbk % 4
                        nbk += 1
                        tm = None
                        for gi, (cb, kind) in enumerate(grp):
                            osl = ps[bank][:, gi * 128:(gi + 1) * 128]
                            special = kind in ("od", "pd")
                            P.op("tensor", lambda e, osl=osl, i=i, cb=cb, j=j: e.matmul(
                                osl, lhsT=kh[i][:, cb * 128:(cb + 1) * 128], rhs=qh[i][:, j * 128:(j + 1) * 128], start=True, stop=False),
                                LDH + [psfree[bank], TC[1], TC[3]])
                            tm = P.op("tensor", lambda e, osl=osl, i=i, cb=cb, j=j, special=special: e.matmul(
                                osl, lhsT=fkl[i][0:2, cb * 128:(cb + 1) * 128], rhs=fkr[i][0:2, j * 128:(j + 1) * 128],
                                start=False, stop=(not special)), [], inc=None if special else "pe")
                            if special:
                                mk = tri_st_b if kind == "od" else pmb[:, par * 128:(par + 1) * 128]
                                tm = P.op("tensor", lambda e, osl=osl, mk=mk: e.matmul(osl, lhsT=ident_b, rhs=mk, start=False, stop=True),
                                          [], inc="pe")
                        pi = npt % 3
                        npt += 1
                        ta = P.op("scalar", lambda e, pi=pi, bank=bank, ng=ng: e.activation(
                            out=PT[pi][:, 0:ng * 128], in_=ps[bank][:, 0:ng * 128], func=AF.Exp), [tm, PT_free[pi]], inc="act")
                        psfree[bank] = ta
                        tm2 = None
                        for gi, (cb, kind) in enumerate(grp):
                            first = (g0 + gi == 0)
                            last = (g0 + gi == len(kl) - 1)
                            P.op("tensor", lambda e, pob=pob, i=i, cb=cb, pi=pi, gi=gi, first=first, last=last: e.matmul(
                                ps[pob][:, 0:128], lhsT=vh[i][:, cb, :], rhs=PT[pi][:, gi * 128:(gi + 1) * 128], start=first, stop=last),
                                [ta, psfree[pob] if first else None])
                            tm2 = P.op("tensor", lambda e, pdb=pdb, pi=pi, gi=gi, first=first, last=last: e.matmul(
                                ps[pdb][:, 0:128], lhsT=ones_b, rhs=PT[pi][:, gi * 128:(gi + 1) * 128], start=first, stop=last),
                                [psfree[pdb] if first else None], inc="pe")
                        PT_free[pi] = tm2
                        tmlast = tm2
                    ta1 = P.op("scalar", lambda e, oi=oi, pob=pob: e.activation(out=of[oi][:, :], in_=ps[pob][:, 0:128], func=AF.Identity),
                               [tmlast, odf_free[oi]], inc="act")
                    ta2 = P.op("scalar", lambda e, oi=oi, pdb=pdb: e.activation(out=df[oi][:, :], in_=ps[pdb][:, 0:128], func=AF.Identity),
                               [tmlast], inc="act")
                    psfree[pob] = ta1
                    psfree[pdb] = ta2
                    tr = P.op("vector", lambda e, oi=oi: e.reciprocal(out=df[oi][:, :], in_=df[oi][:, :]), [ta2], inc="dve")
                    tp = P.op("vector", lambda e, i=i, j=j, oi=oi: e.tensor_tensor(
                        out=ost[i][:, j * 128:(j + 1) * 128], in0=of[oi][:, :], in1=df[oi][:, :], op=ALU.mult),
                        [tr, ta1, ost_free[i]], inc="dve")
                    odf_free[oi] = tp
                ost_free[i] = P.dma("sync", obT[:, h, :], ost[i][:, :], [tp], inc=f"x_st{i}")
                buf_free[i] = tmlast
            P.barrier()

    def dsa(A_all, BT):
        with contextlib.ExitStack() as st:
            def sb(name, shape, dt):
                return st.enter_context(nc.sbuf_tensor(name, list(shape), dt))
            qa_sb = [sb(f"d_q{i}", [128, 4, 1024], BF16) for i in range(2)]
            ka_sb = sb("d_k", [128, 2048], BF16)
            va_sb = sb("d_v", [128, 16, 128], BF16)
            PT = [sb(f"d_pt{i}", [128, 4, 128], BF16) for i in range(3)]
            PT_free = [None] * 3
            of = [sb(f"d_of{i}", [128, 512], F32) for i in range(2)]
            df = [sb(f"d_df{i}", [128, 512], F32) for i in range(2)]
            odf_free = [None, None]
            ost = [sb(f"d_os{i}", [128, 4, 1024], BF16) for i in range(2)]
            ost_free = [None, None]
            lk = P.dma("sync", ka_sb[:, :], kaT, inc="d_lk")
            lv = P.dma("sync", va_sb[:, :, :], va.rearrange("(b p) d -> p b d", p=128), inc="d_lv")
            buf_free = [None, None]
            npt = 0
            nbk = 0
            nn = 0
            for hg in range(4):
                i = hg % 2
                lq = P.dma("sync", qa_sb[i][:, :, :], qaT[:, hg * 4:(hg + 1) * 4, :], waits=[buf_free[i]], inc=f"d_lq{i}")
                tp = None
                tmlast = None
                for j in range(8):
                    par = j % 2
                    kl = kblist(j)
                    pob = 4 + (nn % 2)
                    pdb = 6 + (nn % 2)
                    oi = nn % 2
                    nn += 1
                    for idx, (cb, kind) in enumerate(kl):
                        bank = nbk % 4
                        nbk += 1
                        slot = None
                        if kind is not None:
                            slot = par * 3 + {"od": 0, "op": 1, "pd": 2}[kind]
                        o3 = ps[bank][:, :].rearrange("p (a b) -> p a b", b=128)
                        P.op("tensor", lambda e, o3=o3, cb=cb, i=i, j=j: e.matmul(
                            o3, lhsT=ka_sb[:, cb * 128:(cb + 1) * 128], rhs=qa_sb[i][:, :, j * 128:(j + 1) * 128], start=True, stop=False),
                            [lk, lv, lq, psfree[bank], TC[1]])
                        tm = P.op("tensor", lambda e, o3=o3, j=j, idx=idx, slot=slot: e.matmul(
                            o3, lhsT=ident_b, rhs=A_all[:, ablk[j] + idx, :].unsqueeze(1).broadcast_to([128, 4, 128]),
                            start=False, stop=(slot is None)), [], inc=None if slot is not None else "pe")
                        if slot is not None:
                            tm = P.op("tensor", lambda e, o3=o3, slot=slot, hg=hg: e.matmul(
                                o3, lhsT=ident_b, rhs=BT[:, slot, hg * 4:(hg + 1) * 4, :], start=False, stop=True), [], inc="pe")
                        pi = npt % 3
                        npt += 1
                        ta = P.op("scalar", lambda e, pi=pi, bank=bank: e.activation(
                            out=PT[pi][:, :, :], in_=ps[bank][:, :].rearrange("p (a b) -> p a b", b=128), func=AF.Exp),
                            [tm, PT_free[pi]], inc="act")
                        psfree[bank] = ta
                        first = (idx == 0)
                        last = (idx == len(kl) - 1)
                        P.op("tensor", lambda e, pob=pob, cb=cb, pi=pi, first=first, last=last: e.matmul(
                            ps[pob][:, :].rearrange("p (a b) -> p a b", b=128), lhsT=va_sb[:, cb, :], rhs=PT[pi][:, :, :], start=first, stop=last),
                            [ta, psfree[pob] if first else None])
                        tm2 = P.op("tensor", lambda e, pdb=pdb, pi=pi, first=first, last=last: e.matmul(
                            ps[pdb][:, :].rearrange("p (a b) -> p a b", b=128), lhsT=ones_b, rhs=PT[pi][:, :, :], start=first, stop=last),
                            [psfree[pdb] if first else None], inc="pe")
                        PT_free[pi] = tm2
                        tmlast = tm2
                    ta1 = P.op("scalar", lambda e, oi=oi, pob=pob: e.activation(out=of[oi][:, :], in_=ps[pob][:, :], func=AF.Identity),
                               [tmlast, odf_free[oi]], inc="act")
                    ta2 = P.op("scalar", lambda e, oi=oi, pdb=pdb: e.activation(out=df[oi][:, :], in_=ps[pdb][:, :], func=AF.Identity),
                               [tmlast], inc="act")
                    psfree[pob] = ta1
                    psfree[pdb] = ta2
                    tr = P.op("vector", lambda e, oi=oi: e.reciprocal(out=df[oi][:, :], in_=df[oi][:, :]), [ta2], inc="dve")
                    tp = P.op("vector", lambda e, i=i, j=j, oi=oi: e.tensor_tensor(
                        out=ost[i][:, :, j * 128:(j + 1) * 128], in0=of[oi][:, :].rearrange("p (a b) -> p a b", b=128),
                        in1=df[oi][:, :].rearrange("p (a b) -> p a b", b=128), op=ALU.mult),
                        [tr, ta1, ost_free[i]], inc="dve")
                    odf_free[oi] = tp
                ost_free[i] = P.dma("sync", oaT[:, hg * 4:(hg + 1) * 4, :], ost[i][:, :, :], [tp], inc=f"d_st{i}")
                buf_free[i] = tmlast
            P.barrier()

    def mixer_out():
        with contextlib.ExitStack() as st:
            def sb(name, shape, dt):
                return st.enter_context(nc.sbuf_tensor(name, list(shape), dt))
            oa = sb("o_oa", [128, 16, 512], BF16)
            ob = sb("o_ob", [128, 16, 512], BF16)
            mT = sb("o_mT", [128, 32, 512], BF16)
            ga_s = [sb(f"o_ga{i}", [128, 4, 512], BF16) for i in range(2)]
            gb_s = [sb(f"o_gb{i}", [128, 4, 512], BF16) for i in range(2)]
            g_free = [None, None]
            t1b = [sb(f"o_t1{i}", [128, 512], F32) for i in range(2)]
            t2b = [sb(f"o_t2{i}", [128, 512], F32) for i in range(2)]
            tb_free = [None, None]
            gt = [sb(f"o_gt{i}", [128, 512], F32) for i in range(2)]
            gt_free = [None, None]
            xt = [sb(f"o_xt{i}", [128, 512], F32) for i in range(4)]
            yo = [sb(f"o_yo{i}", [128, 512], F32) for i in range(4)]
            xt_free = [None] * 4
            yo_free = [None] * 4
            nep = 0
            nk = 0
            o_free = None
            m_free = None
            for tt in range(2):
                t0 = tt * 512
                la = P.dma("sync", oa[:, :, :], oaT[:, :, t0:t0 + 512], waits=[o_free], inc="o_la")
                lb = P.dma("sync", ob[:, :, :], obT[:, :, t0:t0 + 512], waits=[o_free], inc="o_lb")
                v3 = None
                tmB = None
                for pc in range(8):
                    i = pc % 2
                    lga = P.dma("sync", ga_s[i][:, :, :], gaT[:, pc * 4:(pc + 1) * 4, t0:t0 + 512], waits=[g_free[i]], inc=f"o_lga{i}")
                    lgb = P.dma("sync", gb_s[i][:, :, :], gbT[:, pc * 4:(pc + 1) * 4, t0:t0 + 512], waits=[g_free[i]], inc=f"o_lgb{i}")
                    for (wsrc, act, boff, ld) in ((w_upa, oa, 0, la), (w_upb, ob, 4, lb)):
                        for hf in range(2):
                            s, tw = wload(wsrc[hf * 1024:(hf + 1) * 1024, pc * 512:(pc + 1) * 512], KS, 512)
                            tm = None
                            for q in range(4):
                                for kc in range(KS):
                                    first = (hf == 0 and kc == 0)
                                    lastk = (hf == 1 and kc == KS - 1)
                                    tm = P.op("tensor", lambda e, s=s, q=q, kc=kc, hf=hf, act=act, first=first, lastk=lastk, bk=boff + q: e.matmul(
                                        ps[bk][:, :], lhsT=wr[s][:, kc, q * 128:(q + 1) * 128], rhs=act[:, hf * KS + kc, :], start=first, stop=lastk),
                                        [tw, ld, psfree[boff + q] if first else None], inc="pe" if kc == KS - 1 else None)
                            wrelease(s, tm)
                        tmB = tm
                    for q in range(4):
                        ch = pc * 4 + q
                        k = nk % 2
                        nk += 1
                        v1 = P.op("vector", lambda e, k=k, q=q, i=i: e.tensor_tensor(out=t1b[k][:, :], in0=ps[q][:, :], in1=ga_s[i][:, q, :], op=ALU.mult),
                                  [tmB, lga, tb_free[k]], inc="dve")
                        v2 = P.op("vector", lambda e, k=k, q=q, i=i: e.tensor_tensor(out=t2b[k][:, :], in0=ps[4 + q][:, :], in1=gb_s[i][:, q, :], op=ALU.mult),
                                  [lgb], inc="dve")
                        psfree[q] = v1
                        psfree[4 + q] = v2
                        v3 = P.op("vector", lambda e, k=k, ch=ch: e.tensor_tensor(out=mT[:, ch, :], in0=t1b[k][:, :], in1=t2b[k][:, :], op=ALU.add),
                                  [v2, m_free], inc="dve")
                        tb_free[k] = v3
                    g_free[i] = v2
                o_free = tmB
                pb_last = None
                for ct in range(8):
                    c0 = ct * 512
                    banks = [0, 1, 2, 3] if ct % 2 == 0 else [4, 5, 6, 7]
                    gs = ct % 2
                    tg_ = P.dma("sync", gt[gs][:, :], modbc[:, 5 * D + c0:5 * D + c0 + 512], waits=[gt_free[gs]], inc=f"o_gld{gs}")
                    tm = None
                    for hf in range(4):
                        s, tw = wload(w_o[hf * 1024:(hf + 1) * 1024, c0:c0 + 512], KS, 512)
                        for tb in range(4):
                            for kc in range(KS):
                                first = (hf == 0 and kc == 0)
                                lastk = (hf == 3 and kc == KS - 1)
                                tm = P.op("tensor", lambda e, s=s, kc=kc, hf=hf, tb=tb, first=first, lastk=lastk, bk=banks[tb]: e.matmul(
                                    ps[bk][:, :], lhsT=mT[:, hf * KS + kc, tb * 128:(tb + 1) * 128], rhs=wr[s][:, kc, :], start=first, stop=lastk),
                                    [tw, v3, psfree[banks[tb]] if first else None], inc="pe" if kc == KS - 1 else None)
                        wrelease(s, tm)
                    pb_last = tm
                    for tb in range(4):
                        se = nep % 4
                        nep += 1
                        r0 = t0 + tb * 128
                        tx = P.dma("sync", xt[se][:, :], x1src[r0:r0 + 128, c0:c0 + 512], waits=[xt_free[se]], inc=f"o_xtl{se}")
                        t1 = P.op("vector", lambda e, se=se, bk=banks[tb], gs=gs: e.tensor_tensor(
                            out=yo[se][:, :], in0=ps[bk][:, :], in1=gt[gs][:, :], op=ALU.mult), [tm, tg_, yo_free[se]], inc="dve")
                        psfree[banks[tb]] = t1
                        t2 = P.op("vector", lambda e, se=se: e.tensor_tensor(out=yo[se][:, :], in0=yo[se][:, :], in1=xt[se][:, :], op=ALU.add),
                                  [t1, tx], inc="dve")
                        xt_free[se] = t2
                        yo_free[se] = P.dma("sync", x2d[r0:r0 + 128, c0:c0 + 512], yo[se][:, :], [t2], inc=f"o_yst{se}")
                    gt_free[gs] = t1
                m_free = pb_last
            P.barrier()

    def final_norm(src):
        with contextlib.ExitStack() as st:
            def sb(name, shape, dt):
                return st.enter_context(nc.sbuf_tensor(name, list(shape), dt))
            gfb = sb("n_g", [128, D], F32)
            xb = [sb(f"n_x{i}", [128, D], F32) for i in range(2)]
            jk = sb("n_j", [128, D], BF16)
            ss = sb("n_ss", [128, 8, 4], F32)
            lg = P.dma("sync", gfb[:, :], g_fin[0:1, :].broadcast_to([128, D]), inc="n_lg")
            x_free = [None, None]
            tq = None
            tjk = None
            for b in range(8):
                i = b % 2
                tl = P.dma("sync", xb[i][:, :], src[b * 128:(b + 1) * 128, :], waits=[x_free[i]], inc=f"n_l{i}")
                t1 = P.op("scalar", lambda e, i=i, b=b: e.activation(out=jk[:, :], in_=xb[i][:, :], func=AF.Square),
                          [tl, tjk], inc="act")
                t1 = P.op("vector", lambda e, b=b: e.reduce_sum(out=ss[:, b, 0:1], in_=jk[:, :], axis=AX.X), [t1], inc="dve")
                tjk = t1
                t2 = P.op("vector", lambda e, b=b: e.tensor_scalar(out=ss[:, b, 1:2], in0=ss[:, b, 0:1], scalar1=1.0 / D, scalar2=EPS,
                                                                   op0=ALU.mult, op1=ALU.add), [t1], inc="dve")
                t2 = P.op("scalar", lambda e, b=b: e.activation(out=ss[:, b, 2:3], in_=ss[:, b, 1:2], func=AF.Sqrt), [t2], inc="act")
                t2 = P.op("vector", lambda e, b=b: e.reciprocal(out=ss[:, b, 3:4], in_=ss[:, b, 2:3]), [t2], inc="dve")
                tq = P.op("vector", lambda e, i=i, b=b: e.scalar_tensor_tensor(out=xb[i][:, :], in0=xb[i][:, :], scalar=ss[:, b, 3:4], in1=gfb[:, :],
                                                                              op0=ALU.mult, op1=ALU.mult), [t2, lg], inc="dve")
                x_free[i] = P.dma("sync", out[b * 128:(b + 1) * 128, :], xb[i][:, :], [tq], inc=f"n_s{i}")
            P.barrier()

    mixer_proj()
    if STAGE <= 2:
        return finish(nc, P, out)
    fcumsum()
    with contextlib.ExitStack() as st_attn:
        A_all = st_attn.enter_context(nc.sbuf_tensor("A_all", [128, 72, 128], BF16))
        BT = st_attn.enter_context(nc.sbuf_tensor("BT", [128, 6, NH, 128], BF16))
        t5tables(BT)
        indexer(A_all)
        if STAGE <= 3:
            return finish(nc, P, out)
        fox()
        dsa(A_all, BT)
    if STAGE <= 4:
        return finish(nc, P, out)
    mixer_out()
    if STAGE <= 5 or TEST == "mix":
        return finish(nc, P, out, copy_from=x2d)
    ffn(x2d, x3d, 2, 2, f2_wi, f2_wo, 8, 0.5)
    final_norm(x3d)
    P.emit()
    return nc


def finish(nc, P, out, copy_from=None):
    if copy_from is not None:
        with nc.sbuf_tensor("fin_t", [128, D], F32) as tbuf:
            tf = None
            for b in range(8):
                t1 = P.dma("sync", tbuf[:, :], copy_from[b * 128:(b + 1) * 128, :], waits=[tf], inc="fin_l")
                tf = P.dma("sync", out[b * 128:(b + 1) * 128, :], tbuf[:, :], waits=[t1], inc="fin_s")
            P.op("sync", lambda e: e.nop(), [tf])
    P.barrier()
    P.emit()
    return nc


def _t5_bucket(dist):
    n = np.maximum(dist, 0)
    nf = np.maximum(n, 16).astype(np.float32)
    large = 16 + (np.log(nf / np.float32(16)) / np.float32(math.log(128 / 16)) * np.float32(16)).astype(np.int32)
    large = np.minimum(large, 31)
    return np.where(n < 16, n, large)


def _core_constants(s):
    own = OWN[s]
    par_ = OWN[1 - s]
    order = own + par_
    cstv = np.zeros((128, 512), np.float32)
    idx = np.arange(128)
    cstv[:, 0:128] = np.eye(128, dtype=np.float32)
    cstv[:, 128:256] = np.where(idx[:, None] > idx[None, :], NEG, 0.0)
    cstv[:, 256:384] = np.where(idx[None, :] > idx[:, None], NEGF, 0.0)
    cstv[:, 384:512] = 1.0
    pm = np.zeros((128, 512), np.float32)
    for par in range(2):
        j = par
        masked = par_[j] > own[j]
        pm[:, par * 128:(par + 1) * 128] = NEG if masked else 0.0
        pm[:, 256 + par * 128:256 + (par + 1) * 128] = NEGF if masked else 0.0
    gpos = np.concatenate([np.arange(128) + 128 * g for g in order])
    L = (gpos[:, None] <= gpos[None, :]).astype(np.float32)
    ohv = np.zeros((6, 33, 128, 128), np.float32)
    for par in range(2):
        j = 2 + par
        gq = own[j]
        for kind, gk in enumerate((own[j], own[j - 1], par_[j])):
            dist = (gq - gk) * 128 + idx[None, :] - idx[:, None]
            bk = _t5_bucket(dist)
            slot = par * 3 + kind
            for b in range(32):
                ohv[slot, b] = ((bk == b) & (dist >= 0)).astype(np.float32)
            ohv[slot, 32] = (dist < 0).astype(np.float32)
    return cstv, pm, L, ohv.reshape(6, 33, 128 * 128)


_NC_CACHE = {}


def kernel(**inputs):
    x = np.asarray(inputs["x"], np.float32)
    c = np.asarray(inputs["c"], np.float32)

    def fm(v):
        return np.ascontiguousarray(np.asarray(v, np.float32).reshape(32, 128).T)

    shared = {
        "w_ada": np.ascontiguousarray(inputs["w_ada"][0]),
        "b_ada": np.ascontiguousarray(inputs["b_ada"][0].reshape(1, -1)),
        "g1_fm": fm(inputs["g_ffn1"][0]), "g2_fm": fm(inputs["g_mix"][0]), "g3_fm": fm(inputs["g_ffn2"][0]),
        "g_fin": np.ascontiguousarray(np.asarray(inputs["g_final"], np.float32).reshape(1, -1)),
        "f1_wi": np.ascontiguousarray(inputs["ffn1_w_in"][0]), "f1_wo": np.ascontiguousarray(inputs["ffn1_w_out"][0]),
        "f2_wi": np.ascontiguousarray(inputs["ffn2_w_in"][0]), "f2_wo": np.ascontiguousarray(inputs["ffn2_w_out"][0]),
        "w_in": np.ascontiguousarray(inputs["w_in"][0]),
        "b_fg": np.ascontiguousarray(inputs["b_forget"][0].reshape(1, -1)),
        "rel_b": np.ascontiguousarray(inputs["rel_bias"]),
        "w_upa": np.ascontiguousarray(inputs["w_up_a"][0]), "w_upb": np.ascontiguousarray(inputs["w_up_b"][0]),
        "w_o": np.ascontiguousarray(inputs["w_o"][0]),
    }
    consts = {s: _core_constants(s) for s in range(2)}
    in_maps = []
    for core in range(8):
        b, s = core // 2, core % 2
        order = OWN[s] + OWN[1 - s]
        xb = x[b].reshape(NB, QB, D)[order].reshape(SEQ, D)
        cstv, pm, L, ohv = consts[s]
        m = dict(shared)
        m.update({"x": np.ascontiguousarray(xb), "c_fm": fm(c[b]), "cst": cstv, "pmk": pm, "Lmat": L, "oh": ohv})
        for kx, vx in inputs.items():
            if kx.startswith("T_"):
                m[kx[2:]] = vx[core]
        in_maps.append(m)
    if "nc" not in _NC_CACHE:
        _NC_CACHE["nc"] = build_program()
    nc = _NC_CACHE["nc"]
    in_maps = [{kx: m[kx] for kx in DECLARED} for m in in_maps]
    ncores = int(os.environ.get("MK_NCORES", "8"))
    res = run_bass_kernel_spmd(nc, in_maps[:ncores], core_ids=list(range(ncores)))
    _NC_CACHE["res"] = res
    outv = np.zeros((4, SEQ, D), np.float32)
    for core in range(ncores):
        b, s = core // 2, core % 2
        o = np.asarray(res.results[core]["out"]).reshape(8, QB, D)
        for j, g in enumerate(OWN[s]):
            outv[b, g * QB:(g + 1) * QB] = o[j]
    return outv
```

```python
import os
import math
import contextlib
import numpy as np
import concourse.bass as bass
import concourse.mybir as mybir
from concourse.bass_utils import run_bass_kernel_spmd

F32 = mybir.dt.float32
BF16 = mybir.dt.bfloat16
AF = mybir.ActivationFunctionType
ALU = mybir.AluOpType
AX = mybir.AxisListType

D = 4096
DFF = 11008
SEQ = 2048
NB = 16
QB = 128
HD = 128
NH = 16
DIN = 17760
EPS = 1e-6
NEG = -30000.0
NEGF = -1.0e30
STAGE = int(os.environ.get("MK_STAGE", "99"))
DEBUG = [v for v in os.environ.get("MK_DEBUG", "").split(",") if v]
TEST = os.environ.get("MK_TEST", "")
DECLARED = []

O_QA, O_KA, O_VA, O_QI, O_KI, O_WI, O_QB, O_KB, O_VB, O_FB, O_GA, O_GB = (
    0, 2048, 2176, 2304, 3328, 3392, 3408, 5456, 7504, 9552, 9568, 13664)

OWN = {0: [0, 3, 4, 7, 8, 11, 12, 15], 1: [1, 2, 5, 6, 9, 10, 13, 14]}


class Prog:
    ENG = ("sync", "scalar", "vector", "gpsimd", "tensor")

    def __init__(self, nc):
        self.nc = nc
        self.q = {k: [] for k in self.ENG}
        self.cnt = {}
        self.waited = {k: {} for k in self.ENG}
        self.sems = {}
        self.pending = {k: [] for k in self.ENG}
        self.last = {}

    def sem(self, name):
        if name not in self.sems:
            self.sems[name] = self.nc.alloc_semaphore(name=name)
            self.cnt[name] = 0
        return name

    def op(self, eng, fn, waits=(), inc=None, amt=1):
        ws = []
        for w in waits:
            if w is None:
                continue
            s, v = w
            if self.waited[eng].get(s, 0) >= v:
                continue
            self.waited[eng][s] = v
            ws.append((self.sems[s], v))
        tok = None
        semh = None
        if inc is not None:
            self.sem(inc)
            self.cnt[inc] += amt
            tok = (inc, self.cnt[inc])
            semh = self.sems[inc]
            if amt == 1:
                self.last[eng] = tok

        def run(e, ws=ws, fn=fn, semh=semh, amt=amt):
            for (sh, v) in ws:
                e.wait_ge(sh, v)
            ins = fn(e)
            if semh is not None:
                ins.then_inc(semh, amt)
        self.q[eng].append(run)
        return tok

    def dma(self, eng, out, in_, waits=(), inc=None, **kw):
        tok = self.op(eng, lambda e: e.dma_start(out=out, in_=in_, **kw), waits, inc, 16)
        if tok is not None:
            self.pending[eng].append(tok)
        return tok

    def barrier(self):
        best = {}
        for e in self.ENG:
            for tok in self.pending[e]:
                if tok is not None and best.get(tok[0], 0) < tok[1]:
                    best[tok[0]] = tok[1]
            self.pending[e] = []
            tok = self.last.get(e)
            if tok is not None and best.get(tok[0], 0) < tok[1]:
                best[tok[0]] = tok[1]
        toks = list(best.items())
        for e in self.ENG:
            self.op(e, lambda en: en.nop(), toks)

    def emit(self):
        with self.nc.Block() as block:
            for name in self.ENG:
                def body(e, name=name):
                    for f in self.q[name]:
                        f(e)
                getattr(block, name)(body)


def build_program():
    nc = bass.Bass("TRN2", target_bir_lowering=False)
    P = Prog(nc)

    def din(name, shape):
        DECLARED.append(name)
        return nc.dram_tensor(name, list(shape), F32, kind="ExternalInput").ap()

    def scratch(name, shape, dt):
        if name in DEBUG:
            return nc.dram_tensor(name, list(shape), dt, kind="ExternalOutput").ap()
        return nc.dram_tensor(name, list(shape), dt).ap()

    x_in = din("x", [SEQ, D])
    c_fm = din("c_fm", [128, 32])
    if not TEST:
        w_ada = din("w_ada", [D, 9 * D])
        b_ada = din("b_ada", [1, 9 * D])
    g1_fm = din("g1_fm", [128, 32])
    g2_fm = din("g2_fm", [128, 32])
    g3_fm = din("g3_fm", [128, 32])
    g_fin = din("g_fin", [1, D])
    if TEST in ("", "ffn"):
        f1_wi = din("f1_wi", [D, 2 * DFF])
        f1_wo = din("f1_wo", [DFF, D])
    if TEST in ("",):
        f2_wi = din("f2_wi", [D, 2 * DFF])
        f2_wo = din("f2_wo", [DFF, D])
    if TEST in ("", "mix"):
        w_in = din("w_in", [D, DIN])
        w_upa = din("w_upa", [2048, D])
        w_upb = din("w_upb", [2048, D])
        w_o = din("w_o", [D, D])
    b_fg = din("b_fg", [1, NH])
    rel_b = din("rel_b", [32, NH])
    cst = din("cst", [128, 512])
    pmk = din("pmk", [128, 512])
    Lmat = din("Lmat", [SEQ, SEQ])
    oh = din("oh", [6, 33, 128 * 128])
    out = nc.dram_tensor("out", [1024, D], F32, kind="ExternalOutput").ap()

    modbc = din("modbc_in", [128, 9 * D]) if TEST else scratch("modbc", [128, 9 * D], F32)
    x1d = scratch("x1d", [SEQ, D], F32)
    x2d = scratch("x2d", [1024, D], F32)
    x3d = scratch("x3d", [1024, D], F32)
    qaT = scratch("qaT", [128, NH, 1024], BF16)
    kaT = scratch("kaT", [128, SEQ], BF16)
    va = scratch("va", [SEQ, 128], BF16)
    qiT = scratch("qiT", [128, 8, 1024], BF16)
    kiT = scratch("kiT", [64, SEQ], BF16)
    wi = scratch("wi", [1024, NH], F32)
    qbT = scratch("qbT", [128, NH, 1024], BF16)
    kbT = scratch("kbT", [128, NH, SEQ], BF16)
    vb = scratch("vb", [SEQ, NH * HD], BF16)
    fb = scratch("fb", [SEQ, NH], F32)
    gaT = scratch("gaT", [128, 32, 1024], BF16)
    gbT = scratch("gbT", [128, 32, 1024], BF16)
    FTd = scratch("FTd", [NH, SEQ], F32)
    nFTd = scratch("nFTd", [NH, SEQ], F32)
    btd = scratch("btd", [6, NH, 128, 128], F32)
    oaT = scratch("oaT", [128, NH, 1024], BF16)
    obT = scratch("obT", [128, NH, 1024], BF16)

    ps = [nc.alloc_psum_tensor(f"ps{i}", [128, 512], F32) for i in range(8)]
    psfree = [None] * 8

    cf = nc.alloc_sbuf_tensor("cf", [128, 512], F32)
    cb = nc.alloc_sbuf_tensor("cb", [128, 512], BF16)
    pmf = nc.alloc_sbuf_tensor("pmf", [128, 512], F32)
    pmb = nc.alloc_sbuf_tensor("pmb", [128, 256], BF16)
    modT = nc.alloc_sbuf_tensor("modT", [128, 9, 32], F32)
    AB = nc.alloc_sbuf_tensor("AB", [128, 6, 32], F32)
    ident_f = cf[:, 0:128]
    ident_b = cb[:, 0:128]
    tri_st_b = cb[:, 128:256]
    tri_ts_f = cf[:, 256:384]
    ones_b = cb[:, 384:512]

    t_c1 = P.dma("sync", cf[:, :], cst, inc="cld_a")
    t_c2 = P.dma("gpsimd", cb[:, :], cst, inc="cld_b")
    t_c3 = P.dma("sync", pmf[:, :], pmk, inc="cld_c")
    t_c4 = P.dma("gpsimd", pmb[:, :], pmk[:, 0:256], inc="cld_d")
    TC = [t_c1, t_c2, t_c3, t_c4]

    NSL = 4
    KS = 8
    wr = [nc.alloc_sbuf_tensor(f"wr{i}", [128, KS, 512], BF16) for i in range(NSL)]
    wr_free = [None] * NSL
    wr_n = [0]

    def wload(dram2d, kc, ncols):
        s = wr_n[0] % NSL
        wr_n[0] += 1
        view = wr[s][:, 0:kc, 0:ncols]
        tok = P.dma("gpsimd", view, dram2d.rearrange("(kc p) n -> p kc n", p=128),
                    waits=[wr_free[s]], inc=f"wl{s}")
        return s, tok

    def wrelease(s, tok):
        wr_free[s] = tok

    if TEST:
        AB_in = din("AB_in", [128, 6 * 32])
        t_ab = P.dma("sync", AB[:, :, :], AB_in.rearrange("p (a b) -> p a b", b=32), inc="abld")
        P.barrier()
    def mod_setup(es, tag):
        def sb(name, shape, dt):
            return es.enter_context(nc.sbuf_tensor(name + tag, list(shape), dt))
        st = {}
        st["cfm"] = sb("cfm", [128, 32], F32)
        st["cact"] = sb("cact", [128, 32], F32)
        st["cbc"] = sb("cbc", [128, 32, 128], BF16)
        st["wm"] = [sb(f"wm{i}", [128, 32, 512], BF16) for i in range(2)]
        st["bab"] = [sb(f"bab{i}", [128, 512], F32) for i in range(2)]
        st["mblk"] = [sb(f"mblk{i}", [128, 512], F32) for i in range(2)]
        st["dtmp"] = sb("dtmp", [128, 4, 128], F32)
        st["gfm"] = sb("gfm", [128, 3, 32], F32)
        cfm, cact, cbc, gfm = st["cfm"], st["cact"], st["cbc"], st["gfm"]
        t = P.dma("sync", cfm[:, :], c_fm, inc="m_ld" + tag)
        t = P.op("scalar", lambda e: e.activation(out=cact[:, :], in_=cfm[:, :], func=AF.Silu), [t], inc="act")
        st["t_cbc"] = P.op("vector", lambda e: e.tensor_copy(out=cbc[:, :, :],
                                                             in_=cact[:, :].unsqueeze(2).broadcast_to([128, 32, 128])),
                           [t], inc="dve")
        tg = P.dma("sync", gfm[:, 0, :], g1_fm, inc="m_ldg" + tag)
        tg = P.dma("sync", gfm[:, 1, :], g2_fm, inc="m_ldg" + tag)
        st["tg"] = P.dma("sync", gfm[:, 2, :], g3_fm, inc="m_ldg" + tag)
        st["wm_free"] = [None, None]
        st["bab_free"] = [None, None]
        st["mblk_free"] = [None, None]
        st["last_dve"] = None
        st["tag"] = tag
        return st

    def mod_block(st, blk, banks):
        s = blk % 2
        bk = banks[s]
        tag = st["tag"]
        wm, bab, mblk, dtmp, cbc = st["wm"], st["bab"], st["mblk"], st["dtmp"], st["cbc"]
        c0 = blk * 512
        tw = P.dma("gpsimd", wm[s][:, :, :], w_ada[:, c0:c0 + 512].rearrange("(kc p) n -> p kc n", p=128),
                   waits=[st["wm_free"][s]], inc=f"wm{s}" + tag)
        tb = P.dma("sync", bab[s][:, :], b_ada[0:1, c0:c0 + 512].broadcast_to([128, 512]),
                   waits=[st["bab_free"][s]], inc=f"bab{s}" + tag)
        tm = None
        for kc in range(32):
            tm = P.op("tensor", lambda e, s=s, kc=kc, bk=bk: e.matmul(ps[bk][:, :], lhsT=cbc[:, kc, :], rhs=wm[s][:, kc, :],
                                                                     start=(kc == 0), stop=(kc == 31)),
                      [tw, st["t_cbc"], psfree[bk]], inc="pe" if kc == 31 else None)
        st["wm_free"][s] = tm
        te = P.op("vector", lambda e, s=s, bk=bk: e.tensor_tensor(out=mblk[s][:, :], in0=ps[bk][:, :], in1=bab[s][:, :], op=ALU.add),
                  [tm, tb, st["mblk_free"][s], st["last_dve"]], inc="dve")
        psfree[bk] = te
        st["bab_free"][s] = te
        v = blk // 8
        if v % 3 != 2:
            ch0 = (blk % 8) * 4
            t1 = P.op("vector", lambda e, s=s: e.tensor_tensor(
                out=dtmp[:, :, :], in0=mblk[s][:, :].rearrange("p (a b) -> p a b", b=128),
                in1=ident_f.unsqueeze(1).broadcast_to([128, 4, 128]), op=ALU.mult), [te, TC[0]], inc="dve")
            te2 = P.op("vector", lambda e, v=v, ch0=ch0: e.tensor_reduce(
                out=modT[:, v, ch0:ch0 + 4], in_=dtmp[:, :, :], axis=AX.X, op=ALU.add), [t1], inc="dve")
            st["last_dve"] = te2
        else:
            st["last_dve"] = te
        st["mblk_free"][s] = P.dma("sync", modbc[:, c0:c0 + 512], mblk[s][:, :], [st["last_dve"]], inc=f"mst{s}" + tag)

    def mod_ab(st, ks):
        gfm = st["gfm"]
        for k in ks:
            t1 = P.op("vector", lambda e, k=k: e.tensor_scalar(out=AB[:, 2 * k, :], in0=modT[:, 3 * k + 1, :],
                                                               scalar1=1.0, scalar2=None, op0=ALU.add),
                      [st["last_dve"], st["tg"]], inc="dve")
            t1 = P.op("vector", lambda e, k=k: e.tensor_tensor(out=AB[:, 2 * k, :], in0=AB[:, 2 * k, :], in1=gfm[:, k, :], op=ALU.mult),
                      [t1], inc="dve")
            st["last_dve"] = P.op("vector", lambda e, k=k: e.tensor_copy(out=AB[:, 2 * k + 1, :], in_=modT[:, 3 * k, :]), [t1], inc="dve")

    NMOD_EARLY = 40
    if not TEST:
        with contextlib.ExitStack() as es:
            st0 = mod_setup(es, "")
            for blk in range(NMOD_EARLY):
                mod_block(st0, blk, (0, 1))
            mod_ab(st0, (0, 1))
            P.barrier()
    if STAGE <= 0:
        return finish(nc, P, out)

    def norm_tile(es, xsrc, row0, k, hT, tagp):
        xb = es["xb"]
        xn = es["xn"]
        ss = es["ss"]
        last = None
        for b in range(4):
            r0 = row0 + b * 128
            tl = P.dma("sync", xb[:, :], xsrc[r0:r0 + 128, :], waits=[es.get("xb_free")], inc="xld")
            t1 = P.op("scalar", lambda e: e.activation(out=xn[:, :], in_=xb[:, :], func=AF.Square),
                      [tl, es.get("xn_free")], inc="act")
            t1 = P.op("vector", lambda e: e.reduce_sum(out=ss[:, 0:1], in_=xn[:, :], axis=AX.X), [t1, es.get("ss_free")], inc="dve")
            t2 = P.op("vector", lambda e: e.tensor_scalar(out=ss[:, 1:2], in0=ss[:, 0:1], scalar1=1.0 / D, scalar2=EPS,
                                                          op0=ALU.mult, op1=ALU.add), [t1], inc="dve")
            t2 = P.op("scalar", lambda e: e.activation(out=ss[:, 2:3], in_=ss[:, 1:2], func=AF.Sqrt), [t2], inc="act")
            t2 = P.op("vector", lambda e: e.reciprocal(out=ss[:, 3:4], in_=ss[:, 2:3]), [t2], inc="dve")
            t3 = P.op("scalar", lambda e: e.activation(out=xn[:, :], in_=xb[:, :], func=AF.Identity, scale=ss[:, 3:4]),
                      [t2], inc="act")
            es["xb_free"] = t3
            es["ss_free"] = t3
            tlast = None
            for g in range(4):
                bank = 4 + (g % 2)
                pv = ps[bank][:, :].bitcast(BF16)
                tt = None
                for c in range(8):
                    ch = g * 8 + c
                    tt = P.op("tensor", lambda e, pv=pv, c=c, ch=ch: e.transpose(
                        out=pv[:, c * 128:(c + 1) * 128], in_=xn[:, ch * 128:(ch + 1) * 128], identity=ident_b),
                        [t3, psfree[bank], TC[1]], inc="pe" if c == 7 else None)
                ta_last = None
                tv_last = None
                for c in range(8):
                    ch = g * 8 + c
                    if g % 2 == 0:
                        tv_last = P.op("vector", lambda e, pv=pv, c=c, ch=ch, b=b: e.tensor_scalar(
                            out=hT[:, ch, b * 128:(b + 1) * 128], in0=pv[:, c * 128:(c + 1) * 128],
                            scalar1=AB[:, 2 * k, ch:ch + 1], scalar2=AB[:, 2 * k + 1, ch:ch + 1],
                            op0=ALU.mult, op1=ALU.add), [tt, es.get("hT_free")], inc="dve")
                    else:
                        ta_last = P.op("scalar", lambda e, pv=pv, c=c, ch=ch, b=b: e.activation(
                            out=hT[:, ch, b * 128:(b + 1) * 128], in_=pv[:, c * 128:(c + 1) * 128],
                            func=AF.Identity, scale=AB[:, 2 * k, ch:ch + 1], bias=AB[:, 2 * k + 1, ch:ch + 1]),
                            [tt, es.get("hT_free")], inc="act")
                psfree[bank] = tv_last if g % 2 == 0 else ta_last
                if g == 2:
                    last_v = tv_last
                if g == 3:
                    last = P.op("vector", lambda e: e.nop(), [ta_last, last_v])
                    last = P.op("vector", lambda e: e.memset(ss[:, 0:1], 0.0), [ta_last, last_v], inc="dve")
            es["xn_free"] = last
        return last

    def ffn(xsrc, xdst, ntiles, k, wi_d, wo_d, gate_v, half):
        with contextlib.ExitStack() as st:
            def sb(name, shape, dt):
                return st.enter_context(nc.sbuf_tensor(name, list(shape), dt))
            hT = sb(f"f{k}_hT", [128, 32, 512], BF16)
            uT = sb(f"f{k}_uT", [128, 86, 512], BF16)
            es = {"xb": sb(f"f{k}_xb", [128, D], F32), "xn": sb(f"f{k}_xn", [128, D], BF16),
                  "ss": sb(f"f{k}_ss", [128, 4], F32)}
            sl = [sb(f"f{k}_sl{i}", [128, 512], F32) for i in range(2)]
            gt = [sb(f"f{k}_gt{i}", [128, 512], F32) for i in range(2)]
            xt = [sb(f"f{k}_xt{i}", [128, 512], F32) for i in range(4)]
            yo = [sb(f"f{k}_yo{i}", [128, 512], F32) for i in range(4)]
            sl_free = [None, None]
            gt_free = [None, None]
            xt_free = [None] * 4
            yo_free = [None] * 4
            nsl = 0
            nep = 0
            u_done = None
            for tt in range(ntiles):
                row0 = tt * 512
                es["hT_free"] = u_done
                th = norm_tile(es, xsrc, row0, k, hT, "f")
                if "n" == os.environ.get("MK_FFN_PARTS", ""):
                    continue
                pa_last = None
                for fb_ in range(43):
                    f0 = fb_ * 256
                    banks = [0, 1, 2, 3] if fb_ % 2 == 0 else [4, 5, 6, 7]
                    for hf in range(4):
                        s, tw = wload(wi_d[hf * 1024:(hf + 1) * 1024, f0:f0 + 256], KS, 256)
                        tw2 = P.dma("gpsimd", wr[s][:, 0:KS, 256:512],
                                    wi_d[hf * 1024:(hf + 1) * 1024, DFF + f0:DFF + f0 + 256].rearrange("(kc p) n -> p kc n", p=128),
                                    waits=[wr_free[s]], inc=f"wl{s}")
                        tm = None
                        for q in range(4):
                            for kc in range(KS):
                                first = (hf == 0 and kc == 0)
                                lastk = (hf == 3 and kc == KS - 1)
                                tm = P.op("tensor", lambda e, s=s, q=q, kc=kc, hf=hf, first=first, lastk=lastk, bk=banks[q]: e.matmul(
                                    ps[bk][:, :], lhsT=wr[s][:, kc, q * 128:(q + 1) * 128], rhs=hT[:, hf * KS + kc, :],
                                    start=first, stop=lastk),
                                    [tw2, tw, th, psfree[banks[q]] if first else None],
                                    inc="pe" if (kc == KS - 1) else None)
                        wrelease(s, tm)
                    pa_last = tm
                    for q in range(2):
                        ssl = nsl % 2
                        nsl += 1
                        ta = P.op("scalar", lambda e, ssl=ssl, bk=banks[q]: e.activation(out=sl[ssl][:, :], in_=ps[bk][:, :], func=AF.Silu),
                                  [tm, sl_free[ssl]], inc="act")
                        tv = P.op("vector", lambda e, ssl=ssl, bk=banks[2 + q], fc=fb_ * 2 + q: e.tensor_tensor(
                            out=uT[:, fc, :], in0=ps[bk][:, :], in1=sl[ssl][:, :], op=ALU.mult),
                            [ta, es.get("uT_free")], inc="dve")
                        sl_free[ssl] = tv
                        psfree[banks[q]] = ta
                        psfree[banks[2 + q]] = tv
                u_ready = tv
                u_done = pa_last
                if "na" == os.environ.get("MK_FFN_PARTS", ""):
                    continue
                pb_last = None
                for ct in range(8):
                    c0 = ct * 512
                    banks = [0, 1, 2, 3] if ct % 2 == 0 else [4, 5, 6, 7]
                    gs = ct % 2
                    tg_ = P.dma("sync", gt[gs][:, :], modbc[:, gate_v * D + c0:gate_v * D + c0 + 512],
                                waits=[gt_free[gs]], inc=f"gld{gs}")
                    tm = None
                    for pc in range(11):
                        fc0 = pc * KS
                        nfc = min(KS, 86 - fc0)
                        s, tw = wload(wo_d[fc0 * 128:(fc0 + nfc) * 128, c0:c0 + 512], nfc, 512)
                        for tb in range(4):
                            for i in range(nfc):
                                fc = fc0 + i
                                tm = P.op("tensor", lambda e, s=s, i=i, fc=fc, tb=tb, bk=banks[tb]: e.matmul(
                                    ps[bk][:, :], lhsT=uT[:, fc, tb * 128:(tb + 1) * 128], rhs=wr[s][:, i, :],
                                    start=(fc == 0), stop=(fc == 85)),
                                    [tw, u_ready, psfree[banks[tb]] if fc == 0 else None],
                                    inc="pe" if i == nfc - 1 else None)
                        wrelease(s, tm)
                    pb_last = tm
                    for tb in range(4):
                        se = nep % 4
                        nep += 1
                        r0 = row0 + tb * 128
                        tx = P.dma("sync", xt[se][:, :], xsrc[r0:r0 + 128, c0:c0 + 512], waits=[xt_free[se]], inc=f"xtl{se}")
                        t1 = P.op("vector", lambda e, se=se, bk=banks[tb], gs=gs: e.scalar_tensor_tensor(
                            out=yo[se][:, :], in0=ps[bk][:, :], scalar=half, in1=gt[gs][:, :], op0=ALU.mult, op1=ALU.mult),
                            [tm, tg_, yo_free[se]], inc="dve")
                        psfree[banks[tb]] = t1
                        t2 = P.op("vector", lambda e, se=se: e.tensor_tensor(out=yo[se][:, :], in0=yo[se][:, :], in1=xt[se][:, :], op=ALU.add),
                                  [t1, tx], inc="dve")
                        xt_free[se] = t2
                        ts = P.dma("sync", xdst[r0:r0 + 128, c0:c0 + 512], yo[se][:, :], [t2], inc=f"yst{se}")
                        yo_free[se] = ts
                    gt_free[gs] = t1
                es["uT_free"] = pb_last
            P.barrier()

    if TEST in ("", "ffn"):
        ffn(x_in, x1d, 1 if TEST else 4, 0, f1_wi, f1_wo, 2, 0.5)
    if STAGE <= 1:
        return finish(nc, P, out, copy_from=None)

    SC = HD ** -0.5
    x1src = x_in if TEST == "mix" else x1d

    def mixer_proj():
        with contextlib.ExitStack() as st:
            def sb(name, shape, dt):
                return st.enter_context(nc.sbuf_tensor(name, list(shape), dt))
            hT = sb("m_hT", [128, 32, 512], BF16)
            es = {"xb": sb("m_xb", [128, D], F32), "xn": sb("m_xn", [128, D], BF16), "ss": sb("m_ss", [128, 4], F32)}
            stg = [sb(f"m_stg{i}", [128, 4, 512], BF16) for i in range(2)]
            stg_free = [None, None]
            stt = [sb(f"m_stt{i}", [128, 512], BF16) for i in range(4)]
            stt_free = [None] * 4
            stf = [sb(f"m_stf{i}", [128, 16], F32) for i in range(4)]
            stf_free = [None] * 4
            cn = {"stg": 0, "stt": 0, "stf": 0, "pc": 0}
            lastmm = [None]

            def fm_piece(col0, ncols, dest, func, scale, th):
                nq = (ncols + 127) // 128
                w = min(128, ncols)
                banks = [0, 1, 2, 3] if cn["pc"] % 2 == 0 else [4, 5, 6, 7]
                cn["pc"] += 1
                tm = None
                for hf in range(4):
                    s, tw = wload(w_in[hf * 1024:(hf + 1) * 1024, col0:col0 + ncols], KS, ncols)
                    for q in range(nq):
                        for kc in range(KS):
                            first = (hf == 0 and kc == 0)
                            lastk = (hf == 3 and kc == KS - 1)
                            tm = P.op("tensor", lambda e, s=s, q=q, kc=kc, hf=hf, first=first, lastk=lastk, bk=banks[q]: e.matmul(
                                ps[bk][0:w, :], lhsT=wr[s][:, kc, q * 128:q * 128 + w], rhs=hT[:, hf * KS + kc, :],
                                start=first, stop=lastk), [tw, th, psfree[banks[q]] if first else None],
                                inc="pe" if kc == KS - 1 else None)
                    wrelease(s, tm)
                lastmm[0] = tm
                i = cn["stg"] % 2
                cn["stg"] += 1
                te = None
                for q in range(nq):
                    te = P.op("scalar", lambda e, i=i, q=q, bk=banks[q]: e.activation(
                        out=stg[i][0:w, q, :], in_=ps[bk][0:w, :], func=func, scale=scale), [tm, stg_free[i]], inc="act")
                    psfree[banks[q]] = te
                stg_free[i] = P.dma("sync", dest, stg[i][0:w, 0:nq, :], [te], inc=f"stgd{i}")

            def tm_piece(col0, ncols, dest_fn, isf32, th):
                banks = [0, 1, 2, 3] if cn["pc"] % 2 == 0 else [4, 5, 6, 7]
                cn["pc"] += 1
                tm = None
                for hf in range(4):
                    s, tw = wload(w_in[hf * 1024:(hf + 1) * 1024, col0:col0 + ncols], KS, ncols)
                    for tb in range(4):
                        for kc in range(KS):
                            first = (hf == 0 and kc == 0)
                            lastk = (hf == 3 and kc == KS - 1)
                            tm = P.op("tensor", lambda e, s=s, tb=tb, kc=kc, hf=hf, first=first, lastk=lastk, bk=banks[tb]: e.matmul(
                                ps[bk][:, 0:ncols], lhsT=hT[:, hf * KS + kc, tb * 128:(tb + 1) * 128], rhs=wr[s][:, kc, 0:ncols],
                                start=first, stop=lastk), [tw, th, psfree[banks[tb]] if first else None],
                                inc="pe" if kc == KS - 1 else None)
                    wrelease(s, tm)
                lastmm[0] = tm
                for tb in range(4):
                    if isf32:
                        i = cn["stf"] % 4
                        cn["stf"] += 1
                        te = P.op("vector", lambda e, i=i, bk=banks[tb]: e.tensor_copy(out=stf[i][:, 0:ncols], in_=ps[bk][:, 0:ncols]),
                                  [tm, stf_free[i]], inc="dve")
                        psfree[banks[tb]] = te
                        stf_free[i] = P.dma("sync", dest_fn(tb), stf[i][:, 0:ncols], [te], inc=f"stfd{i}")
                    else:
                        i = cn["stt"] % 4
                        cn["stt"] += 1
                        te = P.op("scalar", lambda e, i=i, bk=banks[tb]: e.activation(out=stt[i][:, 0:ncols], in_=ps[bk][:, 0:ncols], func=AF.Identity),
                                  [tm, stt_free[i]], inc="act")
                        psfree[banks[tb]] = te
                        stt_free[i] = P.dma("sync", dest_fn(tb), stt[i][:, 0:ncols], [te], inc=f"sttd{i}")

            for tt in range(4):
                own = tt < 2
                t0 = tt * 512
                es["hT_free"] = lastmm[0]
                th = norm_tile(es, x1src, t0, 1, hT, "m")
                fm_piece(O_KA, 128, kaT[:, t0:t0 + 512].unsqueeze(1), AF.Identity, 1.0, th)
                fm_piece(O_KI, 64, kiT[:, t0:t0 + 512].unsqueeze(1), AF.Identity, 1.0, th)
                for pc in range(4):
                    fm_piece(O_KB + pc * 512, 512, kbT[:, pc * 4:(pc + 1) * 4, t0:t0 + 512], AF.Identity, 1.0, th)
                tm_piece(O_VA, 128, lambda tb: va[t0 + tb * 128:t0 + (tb + 1) * 128, :], False, th)
                for pc in range(4):
                    tm_piece(O_VB + pc * 512, 512, lambda tb, pc=pc: vb[t0 + tb * 128:t0 + (tb + 1) * 128, pc * 512:(pc + 1) * 512], False, th)
                tm_piece(O_FB, 16, lambda tb: fb[t0 + tb * 128:t0 + (tb + 1) * 128, :], True, th)
                if own:
                    for pc in range(4):
                        fm_piece(O_QA + pc * 512, 512, qaT[:, pc * 4:(pc + 1) * 4, t0:t0 + 512], AF.Identity, SC, th)
                    for pc in range(2):
                        fm_piece(O_QI + pc * 512, 512, qiT[:, pc * 4:(pc + 1) * 4, t0:t0 + 512], AF.Identity, 1.0, th)
                    tm_piece(O_WI, 16, lambda tb: wi[t0 + tb * 128:t0 + (tb + 1) * 128, :], True, th)
                    for pc in range(4):
                        fm_piece(O_QB + pc * 512, 512, qbT[:, pc * 4:(pc + 1) * 4, t0:t0 + 512], AF.Identity, SC, th)
                    for pc in range(8):
                        fm_piece(O_GA + pc * 512, 512, gaT[:, pc * 4:(pc + 1) * 4, t0:t0 + 512], AF.Sigmoid, 1.0, th)
                    for pc in range(8):
                        fm_piece(O_GB + pc * 512, 512, gbT[:, pc * 4:(pc + 1) * 4, t0:t0 + 512], AF.Sigmoid, 1.0, th)
            P.barrier()

    def fcumsum():
        with contextlib.ExitStack() as st:
            def sb(name, shape, dt):
                return st.enter_context(nc.sbuf_tensor(name, list(shape), dt))
            fbs = sb("c_fb", [128, 16, 16], F32)
            bfg = sb("c_bfg", [128, 16], F32)
            lf = sb("c_lf", [128, 16, 16], F32)
            Lb = [sb(f"c_L{i}", [128, 4, 512], F32) for i in range(2)]
            L_free = [None, None]
            FT = sb("c_FT", [16, 2048], F32)
            nFT = sb("c_nFT", [16, 2048], F32)
            t1 = P.dma("sync", fbs[:, :, :], fb.rearrange("(b p) h -> p b h", p=128), inc="c_ld1")
            t2 = P.dma("sync", bfg[:, :], b_fg[0:1, :].broadcast_to([128, 16]), inc="c_ld2")
            ta = P.op("vector", lambda e: e.tensor_tensor(out=lf[:, :, :], in0=fbs[:, :, :],
                                                          in1=bfg[:, :].unsqueeze(1).broadcast_to([128, 16, 16]), op=ALU.add),
                      [t1, t2], inc="dve")
            ta = P.op("scalar", lambda e: e.activation(out=lf[:, :, :], in_=lf[:, :, :], func=AF.Exp, scale=-1.0), [ta], inc="act")
            ta = P.op("scalar", lambda e: e.activation(out=lf[:, :, :], in_=lf[:, :, :], func=AF.Ln, bias=1.0), [ta], inc="act")
            ta = P.op("vector", lambda e: e.tensor_scalar(out=lf[:, :, :], in0=lf[:, :, :], scalar1=-1.0, scalar2=None, op0=ALU.mult),
                      [ta], inc="dve")
            n = 0
            for tc in range(4):
                tm = None
                for bg in range(4):
                    i = n % 2
                    n += 1
                    tl = P.dma("sync", Lb[i][:, :, :],
                               Lmat[bg * 512:(bg + 1) * 512, tc * 512:(tc + 1) * 512].rearrange("(b p) t -> p b t", p=128),
                               waits=[L_free[i]], inc=f"c_L{i}")
                    for bb in range(4):
                        bs = bg * 4 + bb
                        tm = P.op("tensor", lambda e, i=i, bb=bb, bs=bs, tc=tc: e.matmul(
                            ps[tc][0:16, :], lhsT=lf[:, bs, :], rhs=Lb[i][:, bb, :], start=(bs == 0), stop=(bs == 15)),
                            [tl, ta, psfree[tc] if bs == 0 else None], inc="pe" if bb == 3 else None)
                    L_free[i] = tm
                te = P.op("scalar", lambda e, tc=tc: e.activation(out=FT[:, tc * 512:(tc + 1) * 512], in_=ps[tc][0:16, :], func=AF.Identity),
                          [tm], inc="act")
                te2 = P.op("vector", lambda e, tc=tc: e.tensor_scalar(out=nFT[:, tc * 512:(tc + 1) * 512], in0=ps[tc][0:16, :],
                                                                     scalar1=-1.0, scalar2=None, op0=ALU.mult), [tm, te], inc="dve")
                psfree[tc] = te2
            P.dma("sync", FTd, FT[:, :], [te2], inc="c_st1")
            P.dma("sync", nFTd, nFT[:, :], [te2], inc="c_st2")
            P.barrier()

    def t5tables(BT):
        with contextlib.ExitStack() as st:
            def sb(name, shape, dt):
                return st.enter_context(nc.sbuf_tensor(name, list(shape), dt))
            rb = sb("t_rb", [33, 16], F32)
            rb31 = sb("t_rb31", [32, 16], F32)
            ohb = [sb(f"t_oh{i}", [33, 4096], F32) for i in range(2)]
            oh_free = [None, None]
            st5 = [sb(f"t_st{i}", [16, 512], F32) for i in range(2)]
            st_free = [None, None]
            t1 = P.dma("sync", rb[0:32, :], rel_b, inc="t_ld1")
            t2 = P.dma("sync", rb31[:, :], rel_b[31:32, :].broadcast_to([32, 16]), inc="t_ld2")
            t3 = P.op("vector", lambda e: e.memset(rb[32:33, :], NEG), [], inc="dve")
            t4 = P.op("vector", lambda e: e.tensor_tensor(out=rb[0:32, :], in0=rb[0:32, :], in1=rb31[:, :], op=ALU.subtract),
                      [t1, t2, t3], inc="dve")
            n = 0
            m = 0
            for slot in range(6):
                flat = btd[slot].rearrange("h s t -> h (s t)")
                for pc in range(4):
                    i = n % 2
                    n += 1
                    tl = P.dma("sync", ohb[i][:, :], oh[slot, :, pc * 4096:(pc + 1) * 4096], waits=[oh_free[i]], inc=f"t_oh{i}")
                    tm = None
                    for c in range(8):
                        bk = m % 4
                        j = m % 2
                        m += 1
                        tm = P.op("tensor", lambda e, i=i, c=c, bk=bk: e.matmul(
                            ps[bk][0:16, :], lhsT=rb[:, :], rhs=ohb[i][:, c * 512:(c + 1) * 512], start=True, stop=True),
                            [tl, t4, psfree[bk]], inc="pe")
                        te = P.op("scalar", lambda e, j=j, bk=bk: e.activation(out=st5[j][:, :], in_=ps[bk][0:16, :], func=AF.Identity),
                                  [tm, st_free[j]], inc="act")
                        psfree[bk] = te
                        off = pc * 4096 + c * 512
                        st_free[j] = P.dma("sync", flat[:, off:off + 512], st5[j][:, :], [te], inc=f"t_st{j}")
                    oh_free[i] = tm
            P.barrier()

    def t5load(BT):
        for slot in range(6):
            P.dma("gpsimd", BT[:, slot, :, :], btd[slot].rearrange("h s t -> s h t"), inc="t_bt")
        P.barrier()

    ablk = [sum(2 * (jj + 1) for jj in range(j)) for j in range(8)]

    def indexer(A_all, between=None):
        with contextlib.ExitStack() as st:
            def sb(name, shape, dt):
                return st.enter_context(nc.sbuf_tensor(name, list(shape), dt))
            qi_sb = sb("i_qi", [128, 8, 1024], BF16)
            ki2 = sb("i_ki", [128, 2048], BF16)
            wi_sb = sb("i_wi", [128, 8, 16], F32)
            acc = sb("i_acc", [128, 2048], F32)
            wk = sb("i_wk", [128, 2048], F32)
            tmp = [sb(f"i_tmp{i}", [128, 512], F32) for i in range(2)]
            tmp_free = [None, None]
            m8 = sb("i_m8", [128, 8], F32)
            selb = sb("i_sel", [128, 2048], BF16)
            tl1 = P.dma("sync", qi_sb[:, :, :], qiT, inc="i_ld1")
            tl2 = P.dma("sync", ki2[0:64, :], kiT, inc="i_ld2")
            tl3 = P.dma("sync", ki2[64:128, :], kiT, inc="i_ld3")
            tl4 = P.dma("sync", wi_sb[:, :, :], wi.rearrange("(j p) h -> p j h", p=128), inc="i_ld4")
            LD = [tl1, tl2, tl3, tl4]
            nb = 0
            nt = 0
            acc_tok = None
            sel_free = None
            for j in range(8):
                par = j % 2
                ncol = 2 * (j + 1) * 128
                nown = (j + 1) * 128
                chunks = []
                for (a0, k0, n) in ((0, 0, nown), (nown, 1024, nown)):
                    for c0 in range(0, n, 512):
                        chunks.append((a0 + c0, k0 + c0, min(512, n - c0)))
                for (a0, k0, n) in chunks:
                    for h in range(16):
                        bk = nb % 4
                        nb += 1
                        base = 64 * (h % 2)
                        c = h // 2
                        tm = P.op("tensor", lambda e, bk=bk, base=base, c=c, j=j, k0=k0, n=n: e.matmul(
                            ps[bk][:, 0:n], lhsT=qi_sb[base:base + 64, c, j * 128:(j + 1) * 128], rhs=ki2[base:base + 64, k0:k0 + n],
                            start=True, stop=True), LD + [psfree[bk]], inc="pe")
                        if h == 0:
                            tv = P.op("vector", lambda e, bk=bk, a0=a0, n=n, j=j: e.tensor_scalar(
                                out=acc[:, a0:a0 + n], in0=ps[bk][:, 0:n], scalar1=0.0, scalar2=wi_sb[:, j, 0:1],
                                op0=ALU.max, op1=ALU.mult), [tm, acc_tok], inc="dve")
                            psfree[bk] = tv
                            acc_tok = tv
                        else:
                            ti = nt % 2
                            nt += 1
                            tv = P.op("vector", lambda e, bk=bk, ti=ti, n=n, j=j, h=h: e.tensor_scalar(
                                out=tmp[ti][:, 0:n], in0=ps[bk][:, 0:n], scalar1=0.0, scalar2=wi_sb[:, j, h:h + 1],
                                op0=ALU.max, op1=ALU.mult), [tm, tmp_free[ti]], inc="dve")
                            psfree[bk] = tv
                            tp = P.op("gpsimd", lambda e, ti=ti, a0=a0, n=n: e.tensor_tensor(
                                out=acc[:, a0:a0 + n], in0=acc[:, a0:a0 + n], in1=tmp[ti][:, 0:n], op=ALU.add),
                                [tv, acc_tok], inc="pool")
                            tmp_free[ti] = tp
                            acc_tok = tp
                tp = P.op("gpsimd", lambda e, j=j: e.tensor_tensor(out=acc[:, j * 128:(j + 1) * 128], in0=acc[:, j * 128:(j + 1) * 128],
                                                                   in1=tri_ts_f, op=ALU.add), [acc_tok, TC[0]], inc="pool")
                pc0 = (2 * j + 1) * 128
                tp = P.op("gpsimd", lambda e, pc0=pc0, par=par: e.tensor_tensor(
                    out=acc[:, pc0:pc0 + 128], in0=acc[:, pc0:pc0 + 128], in1=pmf[:, 256 + par * 128:256 + (par + 1) * 128], op=ALU.add),
                    [tp, TC[2]], inc="pool")
                t = P.op("gpsimd", lambda e, ncol=ncol: e.tensor_copy(out=wk[:, 0:ncol], in_=acc[:, 0:ncol]), [tp, sel_free], inc="pool")
                for r in range(32):
                    t = P.op("vector", lambda e, ncol=ncol: e.max(out=m8[:, :], in_=wk[:, 0:ncol]), [t], inc="dve")
                    if r < 31:
                        t = P.op("vector", lambda e, ncol=ncol: e.match_replace(out=wk[:, 0:ncol], in_to_replace=m8[:, :],
                                                                                in_values=wk[:, 0:ncol], imm_value=-3.0e38), [t], inc="dve")
                ts = P.op("vector", lambda e, ncol=ncol: e.tensor_scalar(out=selb[:, 0:ncol], in0=acc[:, 0:ncol], scalar1=m8[:, 7:8],
                                                                         scalar2=NEG, op0=ALU.is_lt, op1=ALU.mult), [t, sel_free], inc="dve")
                acc_tok = ts
                nblk = 2 * (j + 1)
                tlast = None
                for g0 in range(0, nblk, 8):
                    ng = min(8, nblk - g0)
                    bank = 4 + ((g0 // 8) % 2)
                    pv = ps[bank][:, :].bitcast(BF16)
                    tt = None
                    for c in range(ng):
                        kb = g0 + c
                        tt = P.op("tensor", lambda e, pv=pv, c=c, kb=kb: e.transpose(
                            out=pv[:, c * 128:(c + 1) * 128], in_=selb[:, kb * 128:(kb + 1) * 128], identity=ident_b),
                            [ts, psfree[bank], TC[1]], inc="pe" if c == ng - 1 else None)
                    te = P.op("scalar", lambda e, pv=pv, ng=ng, g0=g0, j=j: e.activation(
                        out=A_all[:, ablk[j] + g0:ablk[j] + g0 + ng, :], in_=pv[:, 0:ng * 128].rearrange("p (a b) -> p a b", b=128),
                        func=AF.Identity), [tt], inc="act")
                    psfree[bank] = te
                    tlast = tt
                sel_free = tlast
                if between is not None:
                    between(j)
            if between is not None:
                between(8)
            P.barrier()

    def kblist(j):
        l = []
        for jj in range(j + 1):
            l.append((jj, "od" if jj == j else ("op" if jj == j - 1 else None)))
        for jj in range(j + 1):
            l.append((8 + jj, "pd" if jj == j else None))
        return l

    def fox():
        with contextlib.ExitStack() as st:
            def sb(name, shape, dt):
                return st.enter_context(nc.sbuf_tensor(name, list(shape), dt))
            qh = [sb(f"x_q{i}", [128, 1024], BF16) for i in range(2)]
            kh = [sb(f"x_k{i}", [128, 2048], BF16) for i in range(2)]
            vh = [sb(f"x_v{i}", [128, 16, 128], BF16) for i in range(2)]
            fkl = [sb(f"x_fl{i}", [2, 2048], F32) for i in range(2)]
            fkr = [sb(f"x_fr{i}", [2, 1024], F32) for i in range(2)]
            PT = [sb(f"x_pt{i}", [128, 512], BF16) for i in range(3)]
            PT_free = [None] * 3
            of = [sb(f"x_of{i}", [128, 128], F32) for i in range(2)]
            df = [sb(f"x_df{i}", [128, 128], F32) for i in range(2)]
            odf_free = [None, None]
            ost = [sb(f"x_os{i}", [128, 1024], BF16) for i in range(2)]
            ost_free = [None, None]
            tms = None
            for i in range(2):
                tms = P.op("vector", lambda e, i=i: e.memset(fkl[i][:, :], 1.0), [], inc="dve")
                tms = P.op("vector", lambda e, i=i: e.memset(fkr[i][:, :], 1.0), [], inc="dve")
            buf_free = [tms, tms]
            npt = 0
            nbk = 0
            nn = 0
            for h in range(NH):
                i = h % 2
                w = [buf_free[i]]
                l1 = P.dma("sync", qh[i][:, :], qbT[:, h, :], waits=w, inc=f"x_l{i}")
                l2 = P.dma("sync", kh[i][:, :], kbT[:, h, :], waits=w, inc=f"x_l{i}")
                l3 = P.dma("sync", vh[i][:, :, :], vb[:, h * 128:(h + 1) * 128].rearrange("(b p) d -> p b d", p=128), waits=w, inc=f"x_l{i}")
                l4 = P.dma("sync", fkl[i][1:2, :], nFTd[h:h + 1, :], waits=w, inc=f"x_l{i}")
                l5 = P.dma("sync", fkr[i][0:1, :], FTd[h:h + 1, 0:1024], waits=w, inc=f"x_l{i}")
                LDH = [l5]
                tp = None
                tmlast = None
                for j in range(8):
                    par = j % 2
                    kl = kblist(j)
                    pob = 4 + (nn % 2)
                    pdb = 6 + (nn % 2)
                    oi = nn % 2
                    nn += 1
                    for g0 in range(0, len(kl), 4):
                        grp = kl[g0:g0 + 4]
                        ng = len(grp)
                        bank = nbk % 4
                        nbk += 1
                        tm = None
                        for gi, (cb, kind) in enumerate(grp):
                            osl = ps[bank][:, gi * 128:(gi + 1) * 128]
                            special = kind in ("od", "pd")
                            P.op("tensor", lambda e, osl=osl, i=i, cb=cb, j=j: e.matmul(
                                osl, lhsT=kh[i][:, cb * 128:(cb + 1) * 128], rhs=qh[i][:, j * 128:(j + 1) * 128], start=True, stop=False),
                                LDH + [psfree[bank], TC[1], TC[3]])
                            tm = P.op("tensor", lambda e, osl=osl, i=i, cb=cb, j=j, special=special: e.matmul(
                                osl, lhsT=fkl[i][0:2, cb * 128:(cb + 1) * 128], rhs=fkr[i][0:2, j * 128:(j + 1) * 128],
                                start=False, stop=(not special)), [], inc=None if special else "pe")
                            if special:
                                mk = tri_st_b if kind == "od" else pmb[:, par * 128:(par + 1) * 128]
                                tm = P.op("tensor", lambda e, osl=osl, mk=mk: e.matmul(osl, lhsT=ident_b, rhs=mk, start=False, stop=True),
                                          [], inc="pe")
                        pi = npt % 3
                        npt += 1
                        ta = P.op("scalar", lambda e, pi=pi, bank=bank, ng=ng: e.activation(
                            out=PT[pi][:, 0:ng * 128], in_=ps[bank][:, 0:ng * 128], func=AF.Exp), [tm, PT_free[pi]], inc="act")
                        psfree[bank] = ta
                        tm2 = None
                        for gi, (cb, kind) in enumerate(grp):
                            first = (g0 + gi == 0)
                            last = (g0 + gi == len(kl) - 1)
                            P.op("tensor", lambda e, pob=pob, i=i, cb=cb, pi=pi, gi=gi, first=first, last=last: e.matmul(
                                ps[pob][:, 0:128], lhsT=vh[i][:, cb, :], rhs=PT[pi][:, gi * 128:(gi + 1) * 128], start=first, stop=last),
                                [ta, psfree[pob] if first else None])
                            tm2 = P.op("tensor", lambda e, pdb=pdb, pi=pi, gi=gi, first=first, last=last: e.matmul(
                                ps[pdb][:, 0:128], lhsT=ones_b, rhs=PT[pi][:, gi * 128:(gi + 1) * 128], start=first, stop=last),
                                [psfree[pdb] if first else None], inc="pe")
                        PT_free[pi] = tm2
                        tmlast = tm2
                    ta1 = P.op("scalar", lambda e, oi=oi, pob=pob: e.activation(out=of[oi][:, :], in_=ps[pob][:, 0:128], func=AF.Identity),
                               [tmlast, odf_free[oi]], inc="act")
                    ta2 = P.op("scalar", lambda e, oi=oi, pdb=pdb: e.activation(out=df[oi][:, :], in_=ps[pdb][:, 0:128], func=AF.Identity),
                               [tmlast], inc="act")
                    psfree[pob] = ta1
                    psfree[pdb] = ta2
                    tr = P.op("vector", lambda e, oi=oi: e.reciprocal(out=df[oi][:, :], in_=df[oi][:, :]), [ta2], inc="dve")
                    tp = P.op("vector", lambda e, i=i, j=j, oi=oi: e.tensor_tensor(
                        out=ost[i][:, j * 128:(j + 1) * 128], in0=of[oi][:, :], in1=df[oi][:, :], op=ALU.mult),
                        [tr, ta1, ost_free[i]], inc="dve")
                    odf_free[oi] = tp
                ost_free[i] = P.dma("sync", obT[:, h, :], ost[i][:, :], [tp], inc=f"x_st{i}")
                buf_free[i] = tmlast
            P.barrier()

    def dsa(A_all, BT):
        with contextlib.ExitStack() as st:
            def sb(name, shape, dt):
                return st.enter_context(nc.sbuf_tensor(name, list(shape), dt))
            qa_sb = [sb(f"d_q{i}", [128, 4, 1024], BF16) for i in range(2)]
            ka_sb = sb("d_k", [128, 2048], BF16)
            va_sb = sb("d_v", [128, 16, 128], BF16)
            PT = [sb(f"d_pt{i}", [128, 4, 128], BF16) for i in range(3)]
            PT_free = [None] * 3
            of = [sb(f"d_of{i}", [128, 512], F32) for i in range(2)]
            df = [sb(f"d_df{i}", [128, 512], F32) for i in range(2)]
            odf_free = [None, None]
            ost = [sb(f"d_os{i}", [128, 4, 1024], BF16) for i in range(2)]
            ost_free = [None, None]
            lk = P.dma("sync", ka_sb[:, :], kaT, inc="d_lk")
            lv = P.dma("sync", va_sb[:, :, :], va.rearrange("(b p) d -> p b d", p=128), inc="d_lv")
            buf_free = [None, None]
            npt = 0
            nbk = 0
            nn = 0
            for hg in range(4):
                i = hg % 2
                lq = P.dma("sync", qa_sb[i][:, :, :], qaT[:, hg * 4:(hg + 1) * 4, :], waits=[buf_free[i]], inc=f"d_lq{i}")
                tp = None
                tmlast = None
                for j in range(8):
                    par = j % 2
                    kl = kblist(j)
                    pob = 4 + (nn % 2)
                    pdb = 6 + (nn % 2)
                    oi = nn % 2
                    nn += 1
                    for idx, (cb, kind) in enumerate(kl):
                        bank = nbk % 4
                        nbk += 1
                        slot = None
                        if kind is not None:
                            slot = par * 3 + {"od": 0, "op": 1, "pd": 2}[kind]
                        o3 = ps[bank][:, :].rearrange("p (a b) -> p a b", b=128)
                        P.op("tensor", lambda e, o3=o3, cb=cb, i=i, j=j: e.matmul(
                            o3, lhsT=ka_sb[:, cb * 128:(cb + 1) * 128], rhs=qa_sb[i][:, :, j * 128:(j + 1) * 128], start=True, stop=False),
                            [lk, lv, lq, psfree[bank], TC[1]])
                        tm = P.op("tensor", lambda e, o3=o3, j=j, idx=idx, slot=slot: e.matmul(
                            o3, lhsT=ident_b, rhs=A_all[:, ablk[j] + idx, :].unsqueeze(1).broadcast_to([128, 4, 128]),
                            start=False, stop=(slot is None)), [], inc=None if slot is not None else "pe")
                        if slot is not None:
                            tm = P.op("tensor", lambda e, o3=o3, slot=slot, hg=hg: e.matmul(
                                o3, lhsT=ident_b, rhs=BT[:, slot, hg * 4:(hg + 1) * 4, :], start=False, stop=True), [], inc="pe")
                        pi = npt % 3
                        npt += 1
                        ta = P.op("scalar", lambda e, pi=pi, bank=bank: e.activation(
                            out=PT[pi][:, :, :], in_=ps[bank][:, :].rearrange("p (a b) -> p a b", b=128), func=AF.Exp),
                            [tm, PT_free[pi]], inc="act")
                        psfree[bank] = ta
                        first = (idx == 0)
                        last = (idx == len(kl) - 1)
                        P.op("tensor", lambda e, pob=pob, cb=cb, pi=pi, first=first, last=last: e.matmul(
                            ps[pob][:, :].rearrange("p (a b) -> p a b", b=128), lhsT=va_sb[:, cb, :], rhs=PT[pi][:, :, :], start=first, stop=last),
                            [ta, psfree[pob] if first else None])
                        tm2 = P.op("tensor", lambda e, pdb=pdb, pi=pi, first=first, last=last: e.matmul(
                            ps[pdb][:, :].rearrange("p (a b) -> p a b", b=128), lhsT=ones_b, rhs=PT[pi][:, :, :], start=first, stop=last),
                            [psfree[pdb] if first else None], inc="pe")
                        PT_free[pi] = tm2
                        tmlast = tm2
                    ta1 = P.op("scalar", lambda e, oi=oi, pob=pob: e.activation(out=of[oi][:, :], in_=ps[pob][:, :], func=AF.Identity),
                               [tmlast, odf_free[oi]], inc="act")
                    ta2 = P.op("scalar", lambda e, oi=oi, pdb=pdb: e.activation(out=df[oi][:, :], in_=ps[pdb][:, :], func=AF.Identity),
                               [tmlast], inc="act")
                    psfree[pob] = ta1
                    psfree[pdb] = ta2
                    tr = P.op("vector", lambda e, oi=oi: e.reciprocal(out=df[oi][:, :], in_=df[oi][:, :]), [ta2], inc="dve")
                    tp = P.op("vector", lambda e, i=i, j=j, oi=oi: e.tensor_tensor(
                        out=ost[i][:, :, j * 128:(j + 1) * 128], in0=of[oi][:, :].rearrange("p (a b) -> p a b", b=128),
                        in1=df[oi][:, :].rearrange("p (a b) -> p a b", b=128), op=ALU.mult),
                        [tr, ta1, ost_free[i]], inc="dve")
                    odf_free[oi] = tp
                ost_free[i] = P.dma("sync", oaT[:, hg * 4:(hg + 1) * 4, :], ost[i][:, :, :], [tp], inc=f"d_st{i}")
                buf_free[i] = tmlast
            P.barrier()

    def mixer_out():
        with contextlib.ExitStack() as st:
            def sb(name, shape, dt):
                return st.enter_context(nc.sbuf_tensor(name, list(shape), dt))
            oa = sb("o_oa", [128, 16, 512], BF16)
            ob = sb("o_ob", [128, 16, 512], BF16)
            mT = sb("o_mT", [128, 32, 512], BF16)
            ga_s = [sb(f"o_ga{i}", [128, 4, 512], BF16) for i in range(2)]
            gb_s = [sb(f"o_gb{i}", [128, 4, 512], BF16) for i in range(2)]
            g_free = [None, None]
            t1b = [sb(f"o_t1{i}", [128, 512], F32) for i in range(2)]
            t2b = [sb(f"o_t2{i}", [128, 512], F32) for i in range(2)]
            tb_free = [None, None]
            gt = [sb(f"o_gt{i}", [128, 512], F32) for i in range(2)]
            gt_free = [None, None]
            xt = [sb(f"o_xt{i}", [128, 512], F32) for i in range(4)]
            yo = [sb(f"o_yo{i}", [128, 512], F32) for i in range(4)]
            xt_free = [None] * 4
            yo_free = [None] * 4
            nep = 0
            nk = 0
            o_free = None
            m_free = None
            for tt in range(2):
                t0 = tt * 512
                la = P.dma("sync", oa[:, :, :], oaT[:, :, t0:t0 + 512], waits=[o_free], inc="o_la")
                lb = P.dma("sync", ob[:, :, :], obT[:, :, t0:t0 + 512], waits=[o_free], inc="o_lb")
                v3 = None
                tmB = None
                for pc in range(8):
                    i = pc % 2
                    lga = P.dma("sync", ga_s[i][:, :, :], gaT[:, pc * 4:(pc + 1) * 4, t0:t0 + 512], waits=[g_free[i]], inc=f"o_lga{i}")
                    lgb = P.dma("sync", gb_s[i][:, :, :], gbT[:, pc * 4:(pc + 1) * 4, t0:t0 + 512], waits=[g_free[i]], inc=f"o_lgb{i}")
                    for (wsrc, act, boff, ld) in ((w_upa, oa, 0, la), (w_upb, ob, 4, lb)):
                        for hf in range(2):
                            s, tw = wload(wsrc[hf * 1024:(hf + 1) * 1024, pc * 512:(pc + 1) * 512], KS, 512)
                            tm = None
                            for q in range(4):
                                for kc in range(KS):
                                    first = (hf == 0 and kc == 0)
                                    lastk = (hf == 1 and kc == KS - 1)
                                    tm = P.op("tensor", lambda e, s=s, q=q, kc=kc, hf=hf, act=act, first=first, lastk=lastk, bk=boff + q: e.matmul(
                                        ps[bk][:, :], lhsT=wr[s][:, kc, q * 128:(q + 1) * 128], rhs=act[:, hf * KS + kc, :], start=first, stop=lastk),
                                        [tw, ld, psfree[boff + q] if first else None], inc="pe" if kc == KS - 1 else None)
                            wrelease(s, tm)
                        tmB = tm
                    for q in range(4):
                        ch = pc * 4 + q
                        k = nk % 2
                        nk += 1
                        v1 = P.op("vector", lambda e, k=k, q=q, i=i: e.tensor_tensor(out=t1b[k][:, :], in0=ps[q][:, :], in1=ga_s[i][:, q, :], op=ALU.mult),
                                  [tmB, lga, tb_free[k]], inc="dve")
                        v2 = P.op("vector", lambda e, k=k, q=q, i=i: e.tensor_tensor(out=t2b[k][:, :], in0=ps[4 + q][:, :], in1=gb_s[i][:, q, :], op=ALU.mult),
                                  [lgb], inc="dve")
                        psfree[q] = v1
                        psfree[4 + q] = v2
                        v3 = P.op("vector", lambda e, k=k, ch=ch: e.tensor_tensor(out=mT[:, ch, :], in0=t1b[k][:, :], in1=t2b[k][:, :], op=ALU.add),
                                  [v2, m_free], inc="dve")
                        tb_free[k] = v3
                    g_free[i] = v2
                o_free = tmB
                pb_last = None
                for ct in range(8):
                    c0 = ct * 512
                    banks = [0, 1, 2, 3] if ct % 2 == 0 else [4, 5, 6, 7]
                    gs = ct % 2
                    tg_ = P.dma("sync", gt[gs][:, :], modbc[:, 5 * D + c0:5 * D + c0 + 512], waits=[gt_free[gs]], inc=f"o_gld{gs}")
                    tm = None
                    for hf in range(4):
                        s, tw = wload(w_o[hf * 1024:(hf + 1) * 1024, c0:c0 + 512], KS, 512)
                        for tb in range(4):
                            for kc in range(KS):
                                first = (hf == 0 and kc == 0)
                                lastk = (hf == 3 and kc == KS - 1)
                                tm = P.op("tensor", lambda e, s=s, kc=kc, hf=hf, tb=tb, first=first, lastk=lastk, bk=banks[tb]: e.matmul(
                                    ps[bk][:, :], lhsT=mT[:, hf * KS + kc, tb * 128:(tb + 1) * 128], rhs=wr[s][:, kc, :], start=first, stop=lastk),
                                    [tw, v3, psfree[banks[tb]] if first else None], inc="pe" if kc == KS - 1 else None)
                        wrelease(s, tm)
                    pb_last = tm
                    for tb in range(4):
                        se = nep % 4
                        nep += 1
                        r0 = t0 + tb * 128
                        tx = P.dma("sync", xt[se][:, :], x1src[r0:r0 + 128, c0:c0 + 512], waits=[xt_free[se]], inc=f"o_xtl{se}")
                        t1 = P.op("vector", lambda e, se=se, bk=banks[tb], gs=gs: e.tensor_tensor(
                            out=yo[se][:, :], in0=ps[bk][:, :], in1=gt[gs][:, :], op=ALU.mult), [tm, tg_, yo_free[se]], inc="dve")
                        psfree[banks[tb]] = t1
                        t2 = P.op("vector", lambda e, se=se: e.tensor_tensor(out=yo[se][:, :], in0=yo[se][:, :], in1=xt[se][:, :], op=ALU.add),
                                  [t1, tx], inc="dve")
                        xt_free[se] = t2
                        yo_free[se] = P.dma("sync", x2d[r0:r0 + 128, c0:c0 + 512], yo[se][:, :], [t2], inc=f"o_yst{se}")
                    gt_free[gs] = t1
                m_free = pb_last
            P.barrier()

    def final_norm(src):
        with contextlib.ExitStack() as st:
            def sb(name, shape, dt):
                return st.enter_context(nc.sbuf_tensor(name, list(shape), dt))
            gfb = sb("n_g", [128, D], F32)
            xb = [sb(f"n_x{i}", [128, D], F32) for i in range(2)]
            jk = sb("n_j", [128, D], BF16)
            ss = sb("n_ss", [128, 8, 4], F32)
            lg = P.dma("sync", gfb[:, :], g_fin[0:1, :].broadcast_to([128, D]), inc="n_lg")
            x_free = [None, None]
            tq = None
            tjk = None
            for b in range(8):
                i = b % 2
                tl = P.dma("sync", xb[i][:, :], src[b * 128:(b + 1) * 128, :], waits=[x_free[i]], inc=f"n_l{i}")
                t1 = P.op("scalar", lambda e, i=i, b=b: e.activation(out=jk[:, :], in_=xb[i][:, :], func=AF.Square),
                          [tl, tjk], inc="act")
                t1 = P.op("vector", lambda e, b=b: e.reduce_sum(out=ss[:, b, 0:1], in_=jk[:, :], axis=AX.X), [t1], inc="dve")
                tjk = t1
                t2 = P.op("vector", lambda e, b=b: e.tensor_scalar(out=ss[:, b, 1:2], in0=ss[:, b, 0:1], scalar1=1.0 / D, scalar2=EPS,
                                                                   op0=ALU.mult, op1=ALU.add), [t1], inc="dve")
                t2 = P.op("scalar", lambda e, b=b: e.activation(out=ss[:, b, 2:3], in_=ss[:, b, 1:2], func=AF.Sqrt), [t2], inc="act")
                t2 = P.op("vector", lambda e, b=b: e.reciprocal(out=ss[:, b, 3:4], in_=ss[:, b, 2:3]), [t2], inc="dve")
                tq = P.op("vector", lambda e, i=i, b=b: e.scalar_tensor_tensor(out=xb[i][:, :], in0=xb[i][:, :], scalar=ss[:, b, 3:4], in1=gfb[:, :],
                                                                              op0=ALU.mult, op1=ALU.mult), [t2, lg], inc="dve")
                x_free[i] = P.dma("sync", out[b * 128:(b + 1) * 128, :], xb[i][:, :], [tq], inc=f"n_s{i}")
            P.barrier()

    mixer_proj()
    if STAGE <= 2:
        return finish(nc, P, out)
    fcumsum()
    with contextlib.ExitStack() as st_attn:
        A_all = st_attn.enter_context(nc.sbuf_tensor("A_all", [128, 72, 128], BF16))
        t5tables(None)
        if TEST:
            indexer(A_all)
        else:
            with contextlib.ExitStack() as es2:
                st1 = mod_setup(es2, "b")

                def between(j):
                    if j < 8:
                        for blk in range(NMOD_EARLY + 4 * j, NMOD_EARLY + 4 * (j + 1)):
                            mod_block(st1, blk, (6, 7))
                    else:
                        mod_ab(st1, (2,))
                indexer(A_all, between)
        if STAGE <= 3:
            return finish(nc, P, out)
        BT = st_attn.enter_context(nc.sbuf_tensor("BT", [128, 6, NH, 128], BF16))
        t5load(BT)
        fox()
        dsa(A_all, BT)
    if STAGE <= 4:
        return finish(nc, P, out)
    mixer_out()
    if STAGE <= 5 or TEST == "mix":
        return finish(nc, P, out, copy_from=x2d)
    ffn(x2d, x3d, 2, 2, f2_wi, f2_wo, 8, 0.5)
    final_norm(x3d)
    P.emit()
    return nc


def finish(nc, P, out, copy_from=None):
    if copy_from is not None:
        with nc.sbuf_tensor("fin_t", [128, D], F32) as tbuf:
            tf = None
            for b in range(8):
                t1 = P.dma("sync", tbuf[:, :], copy_from[b * 128:(b + 1) * 128, :], waits=[tf], inc="fin_l")
                tf = P.dma("sync", out[b * 128:(b + 1) * 128, :], tbuf[:, :], waits=[t1], inc="fin_s")
            P.op("sync", lambda e: e.nop(), [tf])
    P.barrier()
    P.emit()
    return nc


def _t5_bucket(dist):
    n = np.maximum(dist, 0)
    nf = np.maximum(n, 16).astype(np.float32)
    large = 16 + (np.log(nf / np.float32(16)) / np.float32(math.log(128 / 16)) * np.float32(16)).astype(np.int32)
    large = np.minimum(large, 31)
    return np.where(n < 16, n, large)


def _core_constants(s):
    own = OWN[s]
    par_ = OWN[1 - s]
    order = own + par_
    cstv = np.zeros((128, 512), np.float32)
    idx = np.arange(128)
    cstv[:, 0:128] = np.eye(128, dtype=np.float32)
    cstv[:, 128:256] = np.where(idx[:, None] > idx[None, :], NEG, 0.0)
    cstv[:, 256:384] = np.where(idx[None, :] > idx[:, None], NEGF, 0.0)
    cstv[:, 384:512] = 1.0
    pm = np.zeros((128, 512), np.float32)
    for par in range(2):
        j = par
        masked = par_[j] > own[j]
        pm[:, par * 128:(par + 1) * 128] = NEG if masked else 0.0
        pm[:, 256 + par * 128:256 + (par + 1) * 128] = NEGF if masked else 0.0
    gpos = np.concatenate([np.arange(128) + 128 * g for g in order])
    L = (gpos[:, None] <= gpos[None, :]).astype(np.float32)
    ohv = np.zeros((6, 33, 128, 128), np.float32)
    for par in range(2):
        j = 2 + par
        gq = own[j]
        for kind, gk in enumerate((own[j], own[j - 1], par_[j])):
            dist = (gq - gk) * 128 + idx[None, :] - idx[:, None]
            bk = _t5_bucket(dist)
            slot = par * 3 + kind
            for b in range(32):
                ohv[slot, b] = ((bk == b) & (dist >= 0)).astype(np.float32)
            ohv[slot, 32] = (dist < 0).astype(np.float32)
    return cstv, pm, L, ohv.reshape(6, 33, 128 * 128)


_NC_CACHE = {}


def kernel(**inputs):
    x = np.asarray(inputs["x"], np.float32)
    c = np.asarray(inputs["c"], np.float32)

    def fm(v):
        return np.ascontiguousarray(np.asarray(v, np.float32).reshape(32, 128).T)

    shared = {
        "w_ada": np.ascontiguousarray(inputs["w_ada"][0]),
        "b_ada": np.ascontiguousarray(inputs["b_ada"][0].reshape(1, -1)),
        "g1_fm": fm(inputs["g_ffn1"][0]), "g2_fm": fm(inputs["g_mix"][0]), "g3_fm": fm(inputs["g_ffn2"][0]),
        "g_fin": np.ascontiguousarray(np.asarray(inputs["g_final"], np.float32).reshape(1, -1)),
        "f1_wi": np.ascontiguousarray(inputs["ffn1_w_in"][0]), "f1_wo": np.ascontiguousarray(inputs["ffn1_w_out"][0]),
        "f2_wi": np.ascontiguousarray(inputs["ffn2_w_in"][0]), "f2_wo": np.ascontiguousarray(inputs["ffn2_w_out"][0]),
        "w_in": np.ascontiguousarray(inputs["w_in"][0]),
        "b_fg": np.ascontiguousarray(inputs["b_forget"][0].reshape(1, -1)),
        "rel_b": np.ascontiguousarray(inputs["rel_bias"]),
        "w_upa": np.ascontiguousarray(inputs["w_up_a"][0]), "w_upb": np.ascontiguousarray(inputs["w_up_b"][0]),
        "w_o": np.ascontiguousarray(inputs["w_o"][0]),
    }
    consts = {s: _core_constants(s) for s in range(2)}
    in_maps = []
    for core in range(8):
        b, s = core // 2, core % 2
        order = OWN[s] + OWN[1 - s]
        xb = x[b].reshape(NB, QB, D)[order].reshape(SEQ, D)
        cstv, pm, L, ohv = consts[s]
        m = dict(shared)
        m.update({"x": np.ascontiguousarray(xb), "c_fm": fm(c[b]), "cst": cstv, "pmk": pm, "Lmat": L, "oh": ohv})
        for kx, vx in inputs.items():
            if kx.startswith("T_"):
                m[kx[2:]] = vx[core]
        in_maps.append(m)
    if "nc" not in _NC_CACHE:
        _NC_CACHE["nc"] = build_program()
    nc = _NC_CACHE["nc"]
    in_maps = [{kx: m[kx] for kx in DECLARED} for m in in_maps]
    ncores = int(os.environ.get("MK_NCORES", "8"))
    res = run_bass_kernel_spmd(nc, in_maps[:ncores], core_ids=list(range(ncores)))
    _NC_CACHE["res"] = res
    outv = np.zeros((4, SEQ, D), np.float32)
    for core in range(ncores):
        b, s = core // 2, core % 2
        o = np.asarray(res.results[core]["out"]).reshape(8, QB, D)
        for j, g in enumerate(OWN[s]):
            outv[b, g * QB:(g + 1) * QB] = o[j]
    return outv
```
